# Optimizing a Trainium2 kernel written in Bass

```python
import jax, jax.numpy as jnp
from jax import lax
import numpy as np

D_MODEL = 1024
BATCH = 4
SEQ = 4096
DEPTH = 2

CHUNK = 64
EPS = 1e-6
ROPE_BASE = 10000.0
RET_HEADS = 4
RET_DIM = 128
RET_VDIM = 128
ATT_HEADS = 8
ATT_DIM = 64
ATT_LEFT_CHUNKS = 8
REL_CLIP = 128
REL_SIZE = CHUNK - 1 + REL_CLIP + 1
SB_HEADS = 16
SB_DIM = 64
SB_BLOCK = 128
N_EXPERTS = 16
N_GROUPS = 4
TOP_K = 2
D_EXPERT = 512

N_EVEN = (DEPTH + 1) // 2
N_ODD = DEPTH // 2
RET_W = RET_HEADS * RET_DIM
RET_VW = RET_HEADS * RET_VDIM
ATT_W = ATT_HEADS * ATT_DIM
EVEN_IN = 2 * RET_W + 2 * RET_VW + 3 * ATT_W
EVEN_MIX = RET_VW + ATT_W
EVEN_SPLITS = (RET_W, 2 * RET_W, 2 * RET_W + RET_VW, 2 * RET_W + 2 * RET_VW,
               2 * RET_W + 2 * RET_VW + ATT_W, 2 * RET_W + 2 * RET_VW + 2 * ATT_W)
SB_W = SB_HEADS * SB_DIM
ODD_IN = 3 * SB_W

kernel_name = "hybrid_retention_chunkattn_stickbreaking_grouped_moe"


def rms_norm(x, gain):
    xf = x.astype(jnp.float32)
    y = xf * lax.rsqrt(jnp.mean(xf * xf, axis=-1, keepdims=True) + EPS)
    return (y * gain.astype(jnp.float32)).astype(x.dtype)


def head_rms(x):
    xf = x.astype(jnp.float32)
    return xf * lax.rsqrt(jnp.mean(xf * xf, axis=-1, keepdims=True) + EPS)


def rotary(x, pos):
    d = x.shape[-1]
    inv = ROPE_BASE ** (-jnp.arange(0, d, 2, dtype=jnp.float32) / d)
    ang = pos.astype(jnp.float32)[:, None] * inv[None, :]
    cos = jnp.cos(ang)[None, :, None, :]
    sin = jnp.sin(ang)[None, :, None, :]
    x1, x2 = jnp.split(x.astype(jnp.float32), 2, axis=-1)
    return jnp.concatenate([x1 * cos - x2 * sin, x1 * sin + x2 * cos], axis=-1).astype(x.dtype)


def retention(q, k, v):
    B_, S, H, dk = q.shape
    dv = v.shape[-1]
    nc = S // CHUNK
    log_g = jnp.log(1.0 - 2.0 ** (-5.0 - jnp.arange(H, dtype=jnp.float32)))
    idx = jnp.arange(CHUNK, dtype=jnp.float32)
    intra_dec = jnp.exp(log_g[:, None, None] * jnp.abs(idx[:, None] - idx[None, :]))
    q_dec = jnp.exp(log_g[:, None] * (idx[None, :] + 1.0))
    k_dec = jnp.exp(log_g[:, None] * (CHUNK - 1.0 - idx[None, :]))
    chunk_dec = jnp.exp(log_g * CHUNK)
    qc = q.astype(jnp.float32).reshape(B_, nc, CHUNK, H, dk)
    kc = k.astype(jnp.float32).reshape(B_, nc, CHUNK, H, dk)
    vc = v.astype(jnp.float32).reshape(B_, nc, CHUNK, H, dv)
    scores = jnp.einsum('bnihd,bnjhd->bnhij', qc, kc) * intra_dec
    o_intra = jnp.einsum('bnhij,bnjhe->bnihe', scores, vc)
    kv = jnp.einsum('bnjhd,hj,bnjhe->bnhde', kc, k_dec, vc)

    def step(state, kv_n):
        return state * chunk_dec[None, :, None, None] + kv_n, state

    init = jnp.zeros((B_, H, dk, dv), jnp.float32)
    _, prev = lax.scan(step, init, jnp.moveaxis(kv, 1, 0))
    prev = jnp.moveaxis(prev, 0, 1)
    o_cross = jnp.einsum('bnihd,hi,bnhde->bnihe', qc, q_dec, prev)
    return (o_intra + o_cross).reshape(B_, S, H, dv)


def chunk_attention(q, k, v, rel_bias):
    B_, S, H, d = q.shape
    nc = S // CHUNK
    band = ATT_LEFT_CHUNKS + 1

    def gather_band(t):
        tc = t.reshape(B_, nc, CHUNK, H, d)
        tp = jnp.pad(tc, ((0, 0), (ATT_LEFT_CHUNKS, 0), (0, 0), (0, 0), (0, 0)))
        return jnp.concatenate([tp[:, j:j + nc] for j in range(band)], axis=2)

    qc = q.reshape(B_, nc, CHUNK, H, d)
    kb = gather_band(k)
    vb = gather_band(v)
    kk = np.arange(band * CHUNK)
    dist = ATT_LEFT_CHUNKS * CHUNK + np.arange(CHUNK)[:, None] - kk[None, :]
    rel_idx = np.clip(dist, -(CHUNK - 1), REL_CLIP) + (CHUNK - 1)
    bias = rel_bias[:, rel_idx].astype(jnp.float32)
    valid = (np.arange(nc)[:, None] - ATT_LEFT_CHUNKS + kk[None, :] // CHUNK) >= 0
    s = jnp.einsum('bnihd,bnkhd->bnhik', qc, kb).astype(jnp.float32) * (d ** -0.5)
    s = s + bias[None, None]
    s = jnp.where(valid[None, :, None, None, :], s, -jnp.inf)
    p = jax.nn.softmax(s, axis=-1)
    o = jnp.einsum('bnhik,bnkhd->bnihd', p, vb.astype(jnp.float32))
    return o.reshape(B_, S, H, d).astype(q.dtype)


def stick_breaking(q, k, v):
    B_, S, H, d = q.shape
    scale = d ** -0.5
    outs = []
    for blk in range(S // SB_BLOCK):
        q0 = blk * SB_BLOCK
        kend = q0 + SB_BLOCK
        z = jnp.einsum('bihd,bjhd->bhij', q[:, q0:kend], k[:, :kend]).astype(jnp.float32) * scale
        causal = jnp.arange(kend)[None, :] < (q0 + jnp.arange(SB_BLOCK))[:, None]
        log_beta = jax.nn.log_sigmoid(z)
        log_1mb = jnp.where(causal, jax.nn.log_sigmoid(-z), 0.0)
        acc = lax.cumsum(log_1mb, axis=3, reverse=True) - log_1mb
        a = jnp.where(causal, jnp.exp(log_beta + acc), 0.0)
        outs.append(jnp.einsum('bhij,bjhd->bihd', a, v[:, :kend].astype(jnp.float32)))
    return jnp.concatenate(outs, axis=1).astype(q.dtype)


def even_mixer(h, w_in, w_out, q_norm_g, k_norm_g, rel_bias, pos):
    B_, S, _ = h.shape
    proj = h @ w_in
    rq, rk, rv, rg, aq, ak, av = jnp.split(proj, EVEN_SPLITS, axis=-1)
    rq = rotary(rq.reshape(B_, S, RET_HEADS, RET_DIM), pos)
    rk = rotary(rk.reshape(B_, S, RET_HEADS, RET_DIM), pos) * (RET_DIM ** -0.5)
    ret = retention(rq, rk, rv.reshape(B_, S, RET_HEADS, RET_VDIM))
    ret = head_rms(ret).reshape(B_, S, RET_VW).astype(h.dtype) * jax.nn.silu(rg)
    aq = rms_norm(aq.reshape(B_, S, ATT_HEADS, ATT_DIM), q_norm_g)
    ak = rms_norm(ak.reshape(B_, S, ATT_HEADS, ATT_DIM), k_norm_g)
    att = chunk_attention(aq, ak, av.reshape(B_, S, ATT_HEADS, ATT_DIM), rel_bias)
    att = att.reshape(B_, S, ATT_W)
    return jnp.concatenate([ret, att], axis=-1) @ w_out


def odd_mixer(h, w_in, w_out):
    B_, S, _ = h.shape
    q, k, v = jnp.split(h @ w_in, 3, axis=-1)
    shp = (B_, S, SB_HEADS, SB_DIM)
    o = stick_breaking(q.reshape(shp), k.reshape(shp), v.reshape(shp))
    return o.reshape(B_, S, SB_W) @ w_out


def grouped_moe(h, router_w, router_b, w_gate, w_up, w_down):
    B_, S, D = h.shape
    t = h.reshape(B_ * S, D)
    scores = jax.nn.sigmoid((t @ router_w).astype(jnp.float32))
    sel = scores + router_b.astype(jnp.float32)
    per_group = N_EXPERTS // N_GROUPS
    group_score = lax.top_k(sel.reshape(-1, N_GROUPS, per_group), TOP_K)[0].sum(-1)
    best_group = jnp.argmax(group_score, axis=-1)
    in_group = (jnp.arange(N_EXPERTS) // per_group)[None, :] == best_group[:, None]
    _, top_idx = lax.top_k(jnp.where(in_group, sel, -jnp.inf), TOP_K)
    top_s = jnp.take_along_axis(scores, top_idx, axis=-1)
    top_w = top_s / jnp.sum(top_s, axis=-1, keepdims=True)
    combine = jnp.einsum('nk,nke->ne', top_w, jax.nn.one_hot(top_idx, N_EXPERTS, dtype=jnp.float32))
    y = jnp.zeros((t.shape[0], D), jnp.float32)
    for e in range(N_EXPERTS):
        he = jax.nn.silu(t @ w_gate[e]) * (t @ w_up[e])
        y = y + combine[:, e:e + 1] * (he @ w_down[e]).astype(jnp.float32)
    return y.reshape(B_, S, D).astype(h.dtype)


def setup_inputs(seed: int = 0) -> dict:
    key = jax.random.key(seed)
    ks = jax.random.split(key, 20)

    def nrm(k, shape, s):
        return jax.random.normal(k, shape, jnp.float32) * s

    return {
        "x": nrm(ks[0], (BATCH, SEQ, D_MODEL), 1.0),
        "c": nrm(ks[1], (BATCH, D_MODEL), 1.0),
        "ada_w": nrm(ks[2], (DEPTH, D_MODEL, 6 * D_MODEL), 0.5 * D_MODEL ** -0.5),
        "ada_b": nrm(ks[3], (DEPTH, 6 * D_MODEL), 0.02),
        "norm1_g": 1.0 + nrm(ks[4], (DEPTH, D_MODEL), 0.02),
        "norm2_g": 1.0 + nrm(ks[5], (DEPTH, D_MODEL), 0.02),
        "even_w_in": nrm(ks[6], (N_EVEN, D_MODEL, EVEN_IN), D_MODEL ** -0.5),
        "even_w_out": nrm(ks[7], (N_EVEN, EVEN_MIX, D_MODEL), EVEN_MIX ** -0.5),
        "att_q_norm_g": 1.0 + nrm(ks[8], (N_EVEN, ATT_DIM), 0.02),
        "att_k_norm_g": 1.0 + nrm(ks[9], (N_EVEN, ATT_DIM), 0.02),
        "att_rel_bias": nrm(ks[10], (N_EVEN, ATT_HEADS, REL_SIZE), 0.2),
        "odd_w_in": nrm(ks[11], (N_ODD, D_MODEL, ODD_IN), D_MODEL ** -0.5),
        "odd_w_out": nrm(ks[12], (N_ODD, SB_W, D_MODEL), SB_W ** -0.5),
        "router_w": nrm(ks[13], (D_MODEL, N_EXPERTS), D_MODEL ** -0.5),
        "router_b": nrm(ks[14], (N_EXPERTS,), 0.01),
        "exp_w_gate": nrm(ks[15], (DEPTH, N_EXPERTS, D_MODEL, D_EXPERT), D_MODEL ** -0.5),
        "exp_w_up": nrm(ks[16], (DEPTH, N_EXPERTS, D_MODEL, D_EXPERT), D_MODEL ** -0.5),
        "exp_w_down": nrm(ks[17], (DEPTH, N_EXPERTS, D_EXPERT, D_MODEL), D_EXPERT ** -0.5),
    }


def reference(x, c, ada_w, ada_b, norm1_g, norm2_g, even_w_in, even_w_out,
              att_q_norm_g, att_k_norm_g, att_rel_bias, odd_w_in, odd_w_out,
              router_w, router_b, exp_w_gate, exp_w_up, exp_w_down):
    S = x.shape[1]
    pos = jnp.arange(S, dtype=jnp.int32)
    c_act = jax.nn.silu(c)
    for layer in range(DEPTH):
        mod = (c_act @ ada_w[layer] + ada_b[layer])[:, None, :]
        sh1, sc1, g1, sh2, sc2, g2 = jnp.split(mod, 6, axis=-1)
        h = rms_norm(x, norm1_g[layer]) * (1.0 + sc1) + sh1
        i = layer // 2
        if layer % 2 == 0:
            mix = even_mixer(h, even_w_in[i], even_w_out[i], att_q_norm_g[i],
                             att_k_norm_g[i], att_rel_bias[i], pos)
        else:
            mix = odd_mixer(h, odd_w_in[i], odd_w_out[i])
        x = x + g1 * mix
        h = rms_norm(x, norm2_g[layer]) * (1.0 + sc2) + sh2
        x = x + g2 * grouped_moe(h, router_w, router_b, exp_w_gate[layer],
                                 exp_w_up[layer], exp_w_down[layer])
    return x
```

```python
from concourse.bass_utils import run_bass_kernel_spmd

import math
import numpy as np
import concourse.bass as bass
import concourse.mybir as mybir
from contextlib import ExitStack

F32 = mybir.dt.float32
BF16 = mybir.dt.bfloat16
ALU = mybir.AluOpType
AF = mybir.ActivationFunctionType
AX = mybir.AxisListType

D = 1024
T = 2048
NT = T // 128
NSLOT = 4
EPS = 1e-6
NEG = -30000.0


class Res:
    __slots__ = ("name", "lw", "rd")

    def __init__(self, name=""):
        self.name = name
        self.lw = None
        self.rd = {}


class Sched:
    ENG = ["pe", "act", "dve", "pool", "sp"]

    def __init__(self, nc, es):
        self.nc = nc
        self.es = es
        self.q = {e: [] for e in self.ENG}
        self.cnt = {}
        self.sem = {}
        self.seen = {e: {} for e in self.ENG}
        self.step = {}
        self.out_streams = []
        for e in self.ENG:
            self._mk(e, 1)

    def _mk(self, name, step):
        self.sem[name] = self.es.enter_context(self.nc.semaphore("s_" + name.replace(":", "_")))
        self.cnt[name] = 0
        self.step[name] = step

    def _waits(self, eng, reads, writes):
        deps = {}

        def add(d):
            if d is None:
                return
            f, i = d
            if f == eng and eng == "pe":
                return
            if deps.get(f, 0) < i:
                deps[f] = i

        for r in reads:
            add(r.lw)
        for w in writes:
            add(w.lw)
            for f, i in w.rd.items():
                add((f, i))
        ws = []
        for f, i in deps.items():
            if self.seen[eng].get(f, 0) >= i:
                continue
            self.seen[eng][f] = i
            ws.append((self.sem[f], i * self.step[f]))
        return ws

    def op(self, eng, fn, reads=(), writes=()):
        ws = self._waits(eng, reads, writes)
        self.cnt[eng] += 1
        idx = self.cnt[eng]
        sem = self.sem[eng]
        for r in reads:
            r.rd[eng] = idx
        for w in writes:
            w.lw = (eng, idx)
            w.rd = {}

        def run(e, ws=ws, fn=fn, sem=sem):
            for s, v in ws:
                e.wait_ge(s, v)
            fn(e).then_inc(sem, 1)

        self.q[eng].append(run)

    def dma(self, qeng, stream, fn, reads=(), writes=()):
        if stream not in self.sem:
            self._mk(stream, 16)
        ws = self._waits(qeng, reads, writes)
        self.cnt[stream] += 1
        idx = self.cnt[stream]
        sem = self.sem[stream]
        for r in reads:
            r.rd[stream] = idx
        for w in writes:
            w.lw = (stream, idx)
            w.rd = {}

        def run(e, ws=ws, fn=fn, sem=sem):
            for s, v in ws:
                e.wait_ge(s, v)
            fn(e).then_inc(sem, 16)

        self.q[qeng].append(run)

    def cc(self, qeng, stream, fn, reads=(), writes=()):
        if stream not in self.sem:
            self._mk(stream, 1)
        ws = self._waits(qeng, reads, writes)
        self.cnt[stream] += 1
        idx = self.cnt[stream]
        sem = self.sem[stream]
        for r in reads:
            r.rd[stream] = idx
        for w in writes:
            w.lw = (stream, idx)
            w.rd = {}

        def run(e, ws=ws, fn=fn, sem=sem):
            for s, v in ws:
                e.wait_ge(s, v)
            fn(e).then_inc(sem)

        self.q[qeng].append(run)

    def final_wait(self, eng, streams):
        ws = [(self.sem[s], self.cnt[s] * self.step[s]) for s in streams if s in self.sem]

        def run(e, ws=ws):
            for s, v in ws:
                e.wait_ge(s, v)

        self.q[eng].append(run)

    def barrier_all(self):
        snap = {k: self.cnt[k] for k in self.cnt}
        for eng in self.ENG:
            ws = []
            for f, i in snap.items():
                if f == eng or i == 0:
                    continue
                if self.seen[eng].get(f, 0) >= i:
                    continue
                self.seen[eng][f] = i
                ws.append((self.sem[f], i * self.step[f]))

            def run(e, ws=ws):
                for s, v in ws:
                    e.wait_ge(s, v)

            self.q[eng].append(run)

    def emit(self):
        q = self.q
        self.q = {e: [] for e in self.ENG}
        with self.nc.Block() as block:
            @block.tensor
            def _(e):
                for f in q["pe"]:
                    f(e)

            @block.scalar
            def _(e):
                for f in q["act"]:
                    f(e)

            @block.vector
            def _(e):
                for f in q["dve"]:
                    f(e)

            @block.gpsimd
            def _(e):
                for f in q["pool"]:
                    f(e)

            @block.sync
            def _(e):
                for f in q["sp"]:
                    f(e)


class Ctx:
    pass


def mk_ctx(nc, es):
    C = Ctx()
    C.nc = nc
    C.es = es
    C.S = Sched(nc, es)
    C.uid = 0
    C.P = []
    C.rP = []
    C.PP = []
    for j in range(4):
        pp = es.enter_context(nc.psum_tensor("pbb%d" % j, [128, 1024], F32))
        C.PP.append(pp)
        for h in range(2):
            C.P.append(pp[:, h * 512:(h + 1) * 512])
            C.rP.append(Res("pb%d" % (2 * j + h)))
    C.ring = es.enter_context(nc.sbuf_tensor("ring", [128, NSLOT, 4096], BF16))
    C.slots = [(C.ring[:, i, :], Res("ring%d" % i), "dma:ring%d" % i) for i in range(NSLOT)]
    C.ring_i = 0
    return C


def sbt(C, es, name, shape, dt):
    C.uid += 1
    return es.enter_context(C.nc.sbuf_tensor("%s_%d" % (name, C.uid), list(shape), dt))


def load_w(C, view, kt, ncols):
    assert kt * ncols <= 4096
    i = C.ring_i % len(C.slots)
    C.ring_i += 1
    base, r, stream = C.slots[i]
    dst = base[:, 0:kt * ncols].rearrange("p (k n) -> p k n", k=kt)
    C.S.dma("pool", stream, lambda e: e.dma_start(out=dst, in_=view), writes=[r])
    return dst, r


def setup_consts(C, cst_bf_d, cst_f_d):
    nc, S, es = C.nc, C.S, C.es
    ncb = cst_bf_d.shape[1]
    ncf = cst_f_d.shape[1]
    C.cb = sbt(C, es, "cstb", [128, ncb], BF16)
    C.cf = sbt(C, es, "cstf", [128, ncf], F32)
    C.rcst = Res("cst")
    S.dma("pool", "dma:cstb", lambda e: e.dma_start(out=C.cb[:], in_=cst_bf_d), writes=[C.rcst])
    S.dma("sp", "dma:cstf", lambda e: e.dma_start(out=C.cf[:], in_=cst_f_d), writes=[C.rcst])


def bank(C, i):
    return C.P[i], C.rP[i]


def phase_load_x(C, x_d, xT, rx, tag):
    S = C.S
    with ExitStack() as es:
        stage = sbt(C, es, "xstage", [128, 2, 1024], F32)
        rst = [Res(), Res()]
        identf = C.cf[:, C.CF_IDENT:C.CF_IDENT + 128]
        for Tt in range(NT):
            b = Tt % 2
            S.dma("sp", "dma:%s%d" % (tag, b),
                  lambda e, b=b, Tt=Tt: e.dma_start(out=stage[:, b, :], in_=x_d[Tt * 128:(Tt + 1) * 128, :]),
                  writes=[rst[b]])
            for half in range(2):
                bk = (Tt * 2 + half) % 4
                P, rP = bank(C, bk)
                for j in range(4):
                    dt = half * 4 + j
                    S.op("pe", lambda e, P=P, j=j, b=b, dt=dt: e.transpose(
                        out=P[:, j * 128:(j + 1) * 128], in_=stage[:, b, dt * 128:(dt + 1) * 128], identity=identf),
                        reads=[rst[b], C.rcst], writes=[rP])
                eng = "act" if half == 0 else "dve"
                outv = xT[:, half * 4:half * 4 + 4, Tt * 128:(Tt + 1) * 128]
                inv = P[:].rearrange("p (a b) -> p a b", a=4)
                wr = [rx[half * 4 + j][Tt] for j in range(4)]
                if eng == "act":
                    S.op("act", lambda e, outv=outv, inv=inv: e.copy(out=outv, in_=inv), reads=[rP], writes=wr)
                else:
                    S.op("dve", lambda e, outv=outv, inv=inv: e.tensor_copy(out=outv, in_=inv), reads=[rP], writes=wr)
        S.barrier_all()
        S.emit()


def phase_store_x(C, xT, rx, out_d):
    S = C.S
    with ExitStack() as es:
        stage = sbt(C, es, "ostage", [128, 2, 1024], F32)
        rst = [Res(), Res()]
        identf = C.cf[:, C.CF_IDENT:C.CF_IDENT + 128]
        for Tt in range(NT):
            b = Tt % 2
            for half in range(2):
                bk = (Tt * 2 + half) % 4
                P, rP = bank(C, bk)
                for j in range(4):
                    dt = half * 4 + j
                    S.op("pe", lambda e, P=P, j=j, dt=dt, Tt=Tt: e.transpose(
                        out=P[:, j * 128:(j + 1) * 128], in_=xT[:, dt, Tt * 128:(Tt + 1) * 128], identity=identf),
                        reads=[rx[dt][Tt], C.rcst], writes=[rP])
                outv = stage[:, b, half * 512:(half + 1) * 512]
                if half == 0:
                    S.op("act", lambda e, outv=outv, P=P: e.copy(out=outv, in_=P[:]), reads=[rP], writes=[rst[b]])
                else:
                    S.op("dve", lambda e, outv=outv, P=P: e.tensor_copy(out=outv, in_=P[:]), reads=[rP], writes=[rst[b]])
            S.dma("sp", "dma:out%d" % b,
                  lambda e, b=b, Tt=Tt: e.dma_start(out=out_d[Tt * 128:(Tt + 1) * 128, :], in_=stage[:, b, :]),
                  reads=[rst[b]])
        S.final_wait("sp", ["dma:out0", "dma:out1"])
        S.barrier_all()
        S.emit()


def alloc_adaln(C, nv):
    es = C.es
    C.modT = sbt(C, es, "modT", [128, 2, 48], F32)
    C.rmod = Res("mod")
    C.vec = sbt(C, es, "vec", [128, nv], F32)
    C.rvec = Res("vec")
    C.Gt = sbt(C, es, "Gt", [128, 2, 2, 8], F32)


def phase_adaln(C, cvec_d, ada_w_d, vecs_d):
    S, es, nc = C.S, C.es, C.nc
    if not hasattr(C, "modT"):
        alloc_adaln(C, vecs_d.shape[1])
    S.dma("sp", "dma:vec", lambda e: e.dma_start(out=C.vec[:], in_=vecs_d), writes=[C.rvec])
    with ExitStack() as ps:
        cv = sbt(C, ps, "cv", [128, 8], F32)
        cact = sbt(C, ps, "cact", [128, 8], BF16)
        rowb = sbt(C, ps, "rowb", [1, 2, 512], F32)
        rcv, rcact, rrow = Res(), Res(), [Res(), Res()]
        S.dma("sp", "dma:cv", lambda e: e.dma_start(out=cv[:], in_=cvec_d), writes=[rcv])
        S.op("act", lambda e: e.activation(out=cact[:], in_=cv[:], func=AF.Silu), reads=[rcv], writes=[rcact])
        one11 = C.cf[0:1, C.CF_ONE:C.CF_ONE + 1]
        for l in range(2):
            wv = ada_w_d[l].rearrange("(kt p) n -> p kt n", p=128)
            Pm, rPm = bank(C, 2 + l)
            for cbk in range(12):
                w, rw = load_w(C, wv[:, :, cbk * 512:(cbk + 1) * 512], 8, 512)
                P, rP = bank(C, cbk % 2)
                rb_ = cbk % 2
                for kt in range(8):
                    S.op("pe", lambda e, P=P, w=w, kt=kt: e.matmul(
                        P[0:1, :], lhsT=cact[:, kt:kt + 1], rhs=w[:, kt, :], start=(kt == 0), stop=(kt == 7)),
                        reads=[rw, rcact], writes=[rP])
                S.op("dve", lambda e, P=P, rb_=rb_: e.tensor_copy(out=rowb[0:1, rb_, :], in_=P[0:1, :]),
                     reads=[rP], writes=[rrow[rb_]])
                for j in range(4):
                    blk = cbk * 4 + j
                    S.op("pe", lambda e, blk=blk, j=j, rb_=rb_, Pm=Pm: e.matmul(
                        Pm[:, blk:blk + 1], lhsT=rowb[0:1, rb_, j * 128:(j + 1) * 128], rhs=one11, start=True, stop=True),
                        reads=[rrow[rb_], C.rcst], writes=[rPm])
            adab = C.vec[:, C.V_ADAB + l * 48:C.V_ADAB + (l + 1) * 48]
            S.op("dve", lambda e, l=l, Pm=Pm, adab=adab: e.tensor_tensor(out=C.modT[:, l, :], in0=Pm[:, 0:48], in1=adab, op=ALU.add),
                 reads=[rPm, C.rvec], writes=[C.rmod])
            for n in range(2):
                sc = C.modT[:, l, (3 * n + 1) * 8:(3 * n + 1) * 8 + 8]
                ng = C.vec[:, C.V_NG + (l * 2 + n) * 8:C.V_NG + (l * 2 + n) * 8 + 8]
                S.op("dve", lambda e, l=l, n=n, sc=sc, ng=ng: e.scalar_tensor_tensor(
                    out=C.Gt[:, l, n, :], in0=sc, scalar=1.0, in1=ng, op0=ALU.add, op1=ALU.mult),
                    reads=[C.rmod, C.rvec], writes=[C.rmod])
        S.barrier_all()
        S.emit()


def mod_col(C, l, j):
    return C.modT[:, l, j * 8:(j + 1) * 8]


def phase_norm(C, xT, rx, G, SH, hT, rh, nblk=4, emit=True):
    S = C.S
    with ExitStack() as es:
        sq = sbt(C, es, "sq", [128, 2, 8, 512], BF16)
        rstd = sbt(C, es, "rstd", [128, 2, 512], F32)
        tmp = sbt(C, es, "ntmp", [128, 2, 512], F32)
        rsq, rrs, rtmp = [Res(), Res()], [Res(), Res()], [Res(), Res()]
        onesm = C.cb[:, C.CB_ONESM:C.CB_ONESM + 128]
        for tb in range(nblk):
            b = tb % 2
            ts = slice(tb * 512, (tb + 1) * 512)
            rxs = [rx[dt][tb * 4 + k] for dt in range(8) for k in range(4)]
            S.op("act", lambda e, b=b, ts=ts: e.activation(out=sq[:, b, :, :], in_=xT[:, :, ts], func=AF.Square),
                 reads=rxs, writes=[rsq[b]])
            P, rP = bank(C, 6 + b)
            for dt in range(8):
                S.op("pe", lambda e, P=P, b=b, dt=dt: e.matmul(P[:], lhsT=onesm, rhs=sq[:, b, dt, :], start=(dt == 0), stop=(dt == 7)),
                     reads=[rsq[b], C.rcst], writes=[rP])
            S.op("act", lambda e, P=P, b=b: e.activation(out=rstd[:, b, :], in_=P[:], func=AF.Ln, bias=C.cf[:, C.CF_EPS:C.CF_EPS + 1]),
                 reads=[rP, C.rcst], writes=[rrs[b]])
            S.op("act", lambda e, b=b: e.activation(out=rstd[:, b, :], in_=rstd[:, b, :], func=AF.Exp, scale=-0.5),
                 reads=[rrs[b]], writes=[rrs[b]])
            for dt in range(8):
                bb = dt % 2
                S.op("dve", lambda e, bb=bb, dt=dt, ts=ts, b=b: e.tensor_tensor(
                    out=tmp[:, bb, :], in0=xT[:, dt, ts], in1=rstd[:, b, :], op=ALU.mult),
                    reads=[rrs[b]] + [rx[dt][tb * 4 + k] for k in range(4)], writes=[rtmp[bb]])
                S.op("act", lambda e, bb=bb, dt=dt, ts=ts: e.activation(
                    out=hT[:, dt, ts], in_=tmp[:, bb, :], func=AF.Identity, scale=G[:, dt:dt + 1], bias=SH[:, dt:dt + 1]),
                    reads=[rtmp[bb], C.rmod], writes=[rh[dt][tb]])
        S.barrier_all()
        if emit:
            S.emit()


BIG = 1.0e4


def phase_router(C, hT, rh, comb, rcomb):
    S = C.S
    with ExitStack() as es:
        rwb = C.cb[:, C.CB_RW:C.CB_RW + 128].rearrange("p (k e) -> p k e", k=8)
        Pr, rPr = bank(C, 7)
        for Tt in range(NT):
            for dt in range(8):
                S.op("pe", lambda e, Tt=Tt, dt=dt: e.matmul(
                    Pr[:, Tt * 16:(Tt + 1) * 16], lhsT=hT[:, dt, Tt * 128:(Tt + 1) * 128], rhs=rwb[:, dt, :],
                    start=(dt == 0), stop=(dt == 7)),
                    reads=[rh[dt][Tt // 4], C.rcst], writes=[rPr])
        n = [0]

        def tmp(shape):
            n[0] += 1
            return sbt(C, es, "rt%d" % n[0], shape, F32)

        rr = Res("router_tmp")
        sc = tmp([128, 256])
        sel = tmp([128, 256])
        S.op("act", lambda e: e.activation(out=sc[:], in_=Pr[:, 0:256], func=AF.Sigmoid), reads=[rPr], writes=[rr])
        rb = C.vec[:, C.V_RB:C.V_RB + 16]
        sc3 = sc[:].rearrange("p (t e) -> p t e", e=16)
        sel3 = sel[:].rearrange("p (t e) -> p t e", e=16)
        sel4 = sel[:].rearrange("p (a k) -> p a k", k=4)

        def dv(fn):
            S.op("dve", fn, reads=[rr, C.rvec], writes=[rr])

        dv(lambda e: e.tensor_tensor(out=sel3, in0=sc3, in1=rb.unsqueeze(1).to_broadcast([128, 16, 16]), op=ALU.add))
        m1 = tmp([128, 64]); m2 = tmp([128, 64]); eq1 = tmp([128, 256]); sel2 = tmp([128, 256])
        eq14 = eq1[:].rearrange("p (a k) -> p a k", k=4)
        sel24 = sel2[:].rearrange("p (a k) -> p a k", k=4)
        dv(lambda e: e.tensor_reduce(out=m1[:], in_=sel4, axis=AX.X, op=ALU.max))
        dv(lambda e: e.tensor_tensor(out=eq14, in0=sel4, in1=m1[:].unsqueeze(2).to_broadcast([128, 64, 4]), op=ALU.is_equal))
        dv(lambda e: e.scalar_tensor_tensor(out=sel2[:], in0=eq1[:], scalar=-BIG, in1=sel[:], op0=ALU.mult, op1=ALU.add))
        dv(lambda e: e.tensor_reduce(out=m2[:], in_=sel24, axis=AX.X, op=ALU.max))
        gs = tmp([128, 64])
        dv(lambda e: e.tensor_tensor(out=gs[:], in0=m1[:], in1=m2[:], op=ALU.add))
        gs3 = gs[:].rearrange("p (t g) -> p t g", g=4)
        gmax = tmp([128, 16]); pen = tmp([128, 64])
        pen3 = pen[:].rearrange("p (t g) -> p t g", g=4)
        dv(lambda e: e.tensor_reduce(out=gmax[:], in_=gs3, axis=AX.X, op=ALU.max))
        dv(lambda e: e.tensor_tensor(out=pen3, in0=gs3, in1=gmax[:].unsqueeze(2).to_broadcast([128, 16, 4]), op=ALU.is_equal))
        dv(lambda e: e.tensor_scalar(out=pen[:], in0=pen[:], scalar1=BIG, scalar2=-BIG, op0=ALU.mult, op1=ALU.add))
        selg = tmp([128, 256])
        selg4 = selg[:].rearrange("p (a k) -> p a k", k=4)
        selg3 = selg[:].rearrange("p (t e) -> p t e", e=16)
        dv(lambda e: e.tensor_tensor(out=selg4, in0=sel4, in1=pen[:].unsqueeze(2).to_broadcast([128, 64, 4]), op=ALU.add))
        t1 = tmp([128, 16]); e1 = tmp([128, 256]); selg2 = tmp([128, 256]); t2 = tmp([128, 16]); e2 = tmp([128, 256])
        e13 = e1[:].rearrange("p (t e) -> p t e", e=16)
        e23 = e2[:].rearrange("p (t e) -> p t e", e=16)
        selg23 = selg2[:].rearrange("p (t e) -> p t e", e=16)
        dv(lambda e: e.tensor_reduce(out=t1[:], in_=selg3, axis=AX.X, op=ALU.max))
        dv(lambda e: e.tensor_tensor(out=e13, in0=selg3, in1=t1[:].unsqueeze(2).to_broadcast([128, 16, 16]), op=ALU.is_equal))
        dv(lambda e: e.scalar_tensor_tensor(out=selg2[:], in0=e1[:], scalar=-BIG, in1=selg[:], op0=ALU.mult, op1=ALU.add))
        dv(lambda e: e.tensor_reduce(out=t2[:], in_=selg23, axis=AX.X, op=ALU.max))
        dv(lambda e: e.tensor_tensor(out=e23, in0=selg23, in1=t2[:].unsqueeze(2).to_broadcast([128, 16, 16]), op=ALU.is_equal))
        dv(lambda e: e.tensor_tensor(out=e1[:], in0=e1[:], in1=e2[:], op=ALU.add))
        dv(lambda e: e.tensor_tensor(out=e1[:], in0=e1[:], in1=sc[:], op=ALU.mult))
        den = tmp([128, 16])
        dv(lambda e: e.tensor_reduce(out=den[:], in_=e13, axis=AX.X, op=ALU.add))
        dv(lambda e: e.reciprocal(out=den[:], in_=den[:]))
        S.op("dve", lambda e: e.tensor_tensor(out=comb[:], in0=e13, in1=den[:].unsqueeze(2).to_broadcast([128, 16, 16]), op=ALU.mult),
             reads=[rr], writes=[rcomb])
        S.barrier_all()
        S.emit()


def phase_moe(C, l, wg_d, wu_d, wd_d, hT, rh, xT, rx, comb, rcomb, g2):
    S = C.S
    with ExitStack() as es:
        cbt = sbt(C, es, "cbt", [128, 2, T], BF16)
        rcb = [Res(), Res()]
        sgt = sbt(C, es, "sgt", [128, 2, 512], BF16)
        rsg = [Res(), Res()]
        tt = sbt(C, es, "tt", [128, 2, 512], BF16)
        rtt = [Res(), Res()]
        hw = sbt(C, es, "hw", [128, 2, 4, 512], BF16)
        rhw = [[Res() for _ in range(4)] for _ in range(2)]
        identb = C.cb[:, C.CB_IDENT:C.CB_IDENT + 128]
        cnt_gu = 0
        cnt_d = 0
        cnt_hw = 0
        pending = [None]
        for ex in range(16):
            cb_i = ex % 2
            Pc, rPc = bank(C, 6)
            for q4 in range(4):
                for j in range(4):
                    Tt = q4 * 4 + j
                    S.op("pe", lambda e, Tt=Tt, j=j, ex=ex: e.matmul(
                        Pc[:, j * 128:(j + 1) * 128], lhsT=comb[:, Tt, ex:ex + 1].to_broadcast([128, 128]), rhs=identb,
                        start=True, stop=True), reads=[rcomb, C.rcst], writes=[rPc])
                S.op("act", lambda e, q4=q4, cb_i=cb_i: e.copy(out=cbt[:, cb_i, q4 * 512:(q4 + 1) * 512], in_=Pc[:]),
                     reads=[rPc], writes=[rcb[cb_i]])
            wg, rwg = load_w(C, wg_d[ex].rearrange("(kt p) n -> p kt n", p=128), 8, 512)
            wu, rwu = load_w(C, wu_d[ex].rearrange("(kt p) n -> p kt n", p=128), 8, 512)
            wd, rwd = load_w(C, wd_d[ex].rearrange("(kt p) n -> p kt n", p=128), 4, 1024)
            for tb in range(4):
                ts = slice(tb * 512, (tb + 1) * 512)
                hb = cnt_hw % 2
                cnt_hw += 1
                for f in range(4):
                    gb = cnt_gu % 2
                    cnt_gu += 1
                    Pg, rPg = bank(C, gb * 2)
                    Pu, rPu = bank(C, gb * 2 + 1)
                    for kt in range(8):
                        S.op("pe", lambda e, Pg=Pg, wg=wg, kt=kt, f=f, ts=ts: e.matmul(
                            Pg[:], lhsT=wg[:, kt, f * 128:(f + 1) * 128], rhs=hT[:, kt, ts], start=(kt == 0), stop=(kt == 7)),
                            reads=[rwg, rh[kt][tb]], writes=[rPg])
                    for kt in range(8):
                        S.op("pe", lambda e, Pu=Pu, wu=wu, kt=kt, f=f, ts=ts: e.matmul(
                            Pu[:], lhsT=wu[:, kt, f * 128:(f + 1) * 128], rhs=hT[:, kt, ts], start=(kt == 0), stop=(kt == 7)),
                            reads=[rwu, rh[kt][tb]], writes=[rPu])
                    S.op("act", lambda e, Pg=Pg, gb=gb: e.activation(out=sgt[:, gb, :], in_=Pg[:], func=AF.Silu),
                         reads=[rPg], writes=[rsg[gb]])
                    S.op("dve", lambda e, Pu=Pu, gb=gb: e.tensor_tensor(out=tt[:, gb, :], in0=Pu[:], in1=sgt[:, gb, :], op=ALU.mult),
                         reads=[rPu, rsg[gb]], writes=[rtt[gb]])
                    S.op("dve", lambda e, gb=gb, hb=hb, f=f, cb_i=cb_i, ts=ts: e.tensor_tensor(
                        out=hw[:, hb, f, :], in0=tt[:, gb, :], in1=cbt[:, cb_i, ts], op=ALU.mult),
                        reads=[rtt[gb], rcb[cb_i]], writes=[rhw[hb][f]])
                def down_unit(wd=wd, rwd=rwd, hb=hb, tb=tb, ts=ts):
                    nonlocal cnt_d
                    for dt in range(8):
                        db = cnt_d % 2
                        cnt_d += 1
                        Pd, rPd = bank(C, 4 + db)
                        for f in range(4):
                            S.op("pe", lambda e, Pd=Pd, wd=wd, f=f, dt=dt, hb=hb: e.matmul(
                                Pd[:], lhsT=wd[:, f, dt * 128:(dt + 1) * 128], rhs=hw[:, hb, f, :], start=(f == 0), stop=(f == 3)),
                                reads=[rwd, rhw[hb][f]], writes=[rPd])
                        rxs = [rx[dt][tb * 4 + k] for k in range(4)]
                        S.op("dve", lambda e, Pd=Pd, dt=dt, ts=ts: e.scalar_tensor_tensor(
                            out=xT[:, dt, ts], in0=Pd[:], scalar=g2[:, dt:dt + 1], in1=xT[:, dt, ts], op0=ALU.mult, op1=ALU.add),
                            reads=[rPd, C.rmod] + rxs, writes=rxs)

                if pending[0] is not None:
                    pending[0]()
                pending[0] = down_unit
        if pending[0] is not None:
            pending[0]()
        S.barrier_all()
        S.emit()


SEQ = 4096
NTF = SEQ // 128


def load_hblk(C, hfull_d, tb, hblk, rhblk, cnt):
    b = cnt[0] % 2
    cnt[0] += 1
    h_all, rhall = hfull_d
    r_, tl = tb // 4, tb % 4
    r0 = (((tl // 2) * 2 + r_) * 2 + tl % 2) * 128
    C.S.dma("sp", "dma:hblk%d" % b, lambda e, b=b, r0=r0: e.dma_start(out=hblk[:, b, :], in_=h_all[r0:r0 + 128, :]),
            reads=[rhall], writes=[rhblk[b]])
    return hblk[:, b, :].rearrange("p (k n) -> p k n", k=8), rhblk[b]


def rstd_from_ps(C, P, rP, out, rout):
    S = C.S
    S.op("act", lambda e: e.activation(out=out, in_=P, func=AF.Ln, bias=C.cf[:, C.CF_EPS:C.CF_EPS + 1]),
         reads=[rP, C.rcst], writes=[rout])
    S.op("act", lambda e: e.activation(out=out, in_=out, func=AF.Exp, scale=-0.5), reads=[rout], writes=[rout])


def proj_fm(C, w, rw, c0, M, hb, rhb, P, rP):
    for kt in range(8):
        C.S.op("pe", lambda e, kt=kt: e.matmul(P[0:M, :], lhsT=w[:, kt, c0:c0 + M], rhs=hb[:, kt, :], start=(kt == 0), stop=(kt == 7)),
               reads=[rw, rhb], writes=[rP])


def proj_tok(C, w, rw, c0, N, hb, rhb, j, P, rP):
    for kt in range(8):
        C.S.op("pe", lambda e, kt=kt: e.matmul(P[:, 0:N], lhsT=hb[:, kt, j * 128:(j + 1) * 128], rhs=w[:, kt, c0:c0 + N], start=(kt == 0), stop=(kt == 7)),
               reads=[rw, rhb], writes=[rP])


def phase_ret(C, hfull_d, wret_d, tab_d, out_d):
    S = C.S
    with ExitStack() as es:
        qT = sbt(C, es, "qT", [128, 2, SEQ], BF16)
        kT = sbt(C, es, "kT", [128, 2, SEQ], BF16)
        sgT = sbt(C, es, "sgT", [128, 2, SEQ], BF16)
        v_tok = sbt(C, es, "vtok", [128, NTF, 2, 128], BF16)
        rq_ = [[Res() for _ in range(8)] for _ in range(2)]
        rk_ = [[Res() for _ in range(8)] for _ in range(2)]
        rsg_ = [[Res() for _ in range(8)] for _ in range(2)]
        rvt = [[Res() for _ in range(NTF)] for _ in range(2)]
        with ExitStack() as e1:
            hblk = sbt(C, e1, "hblk", [128, 2, 4096], BF16)
            rhblk = [Res(), Res()]
            hcnt = [0]
            cs = sbt(C, e1, "cs", [128, 2, 2, 512], F32)
            rcs = [Res(), Res()]
            t1 = sbt(C, e1, "rt1", [128, 2, 512], F32)
            t2 = sbt(C, e1, "rt2", [128, 2, 512], F32)
            rt1, rt2 = [Res(), Res()], [Res(), Res()]
            ccnt = 0
            for h2 in range(2):
                wv = wret_d[h2].rearrange("(kt p) n -> p kt n", p=128)
                wA, rwA = load_w(C, wv[:, :, 0:384], 8, 384)
                wB, rwB = load_w(C, wv[:, :, 384:768], 8, 384)

                def wcol(g, wA=wA, rwA=rwA, wB=wB, rwB=rwB):
                    if g < 3:
                        return wA, rwA, g * 128
                    return wB, rwB, (g - 3) * 128

                for tb in range(8):
                    hb, rhb = load_hblk(C, hfull_d, tb, hblk, rhblk, hcnt)
                    cb_ = ccnt % 2
                    ccnt += 1
                    ts = slice(tb * 512, (tb + 1) * 512)
                    for ci, nm in enumerate(("cosT", "sinT")):
                        src = tab_d[nm][:, ts]
                        S.dma("sp", "dma:cs%d" % cb_, lambda e, cb_=cb_, ci=ci, src=src: e.dma_start(out=cs[:, cb_, ci, :], in_=src), writes=[rcs[cb_]])
                    for which in range(2):
                        Pa, rPa = bank(C, 0 + 2 * which)
                        Pb, rPb = bank(C, 1 + 2 * which)
                        w, rw, c0 = wcol(2 * which)
                        proj_fm(C, w, rw, c0, 128, hb, rhb, Pa, rPa)
                        w, rw, c0 = wcol(2 * which + 1)
                        proj_fm(C, w, rw, c0, 128, hb, rhb, Pb, rPb)
                        bb = which
                        S.op("dve", lambda e, Pa=Pa, bb=bb, cb_=cb_: e.tensor_tensor(out=t1[:, bb, :], in0=Pa[:], in1=cs[:, cb_, 0, :], op=ALU.mult),
                             reads=[rPa, rcs[cb_]], writes=[rt1[bb]])
                        S.op("dve", lambda e, Pb=Pb, bb=bb, cb_=cb_: e.tensor_tensor(out=t2[:, bb, :], in0=Pb[:], in1=cs[:, cb_, 1, :], op=ALU.mult),
                             reads=[rPb, rcs[cb_]], writes=[rt2[bb]])
                        dst = qT if which == 0 else kT
                        rdst = rq_ if which == 0 else rk_
                        S.op("dve", lambda e, bb=bb, h2=h2, ts=ts, dst=dst: e.tensor_tensor(out=dst[:, h2, ts], in0=t1[:, bb, :], in1=t2[:, bb, :], op=ALU.add),
                             reads=[rt1[bb], rt2[bb]], writes=[rdst[h2][tb]])
                    Pg, rPg = bank(C, 4)
                    w, rw, c0 = wcol(4)
                    proj_fm(C, w, rw, c0, 128, hb, rhb, Pg, rPg)
                    S.op("act", lambda e, h2=h2, ts=ts, Pg=Pg: e.activation(out=sgT[:, h2, ts], in_=Pg[:], func=AF.Silu), reads=[rPg], writes=[rsg_[h2][tb]])
                    for j in range(4):
                        Tt = tb * 4 + j
                        Pt, rPt = bank(C, 5 + (j % 2))
                        w, rw, c0 = wcol(5)
                        proj_tok(C, w, rw, c0, 128, hb, rhb, j, Pt, rPt)
                        S.op("act", lambda e, Tt=Tt, h2=h2, Pt=Pt: e.copy(out=v_tok[:, Tt, h2, :], in_=Pt[:, 0:128]), reads=[rPt], writes=[rvt[h2][Tt]])
            S.barrier_all()
            S.emit()
        with ExitStack() as e2:
            base4 = sbt(C, e2, "base4", [128, 2, 512], F32)
            diag = sbt(C, e2, "diag", [128, 2, 4, 512], F32)
            rtab = Res()
            S.dma("sp", "dma:base4", lambda e: e.dma_start(out=base4[:], in_=tab_d["base4"].rearrange("h p n -> p h n")), writes=[rtab])
            for h2 in range(2):
                S.dma("sp", "dma:diag", lambda e, h2=h2: e.dma_start(out=diag[:, h2, :, :], in_=tab_d["diag"][h2].rearrange("r p n -> p r n")), writes=[rtab])
            AT = sbt(C, e2, "AT", [128, 2, 512], BF16)
            rAT = [Res(), Res()]
            sq = sbt(C, e2, "rsq", [128, 2, 512], BF16)
            rsq = [Res(), Res()]
            rs_ = sbt(C, e2, "rrs", [128, 2, 512], F32)
            rrs = [Res(), Res()]
            ost = sbt(C, e2, "ost", [128, 2, 512], F32)
            rost = [Res(), Res()]
            ones128 = C.cb[:, C.CB_ONES128:C.CB_ONES128 + 128]
            gpow = C.cf[:, C.CF_GPOW:C.CF_GPOW + 64].rearrange("p (h r) -> p h r", h=2)
            acnt = 0
            ncnt = 0
            for qb in range(8):
                ts = slice(qb * 512, (qb + 1) * 512)
                for h2 in range(2):
                    Po, rPo = bank(C, ncnt % 2)
                    nk = 4 * qb + 4

                    def r_qk(kt):
                        nonlocal acnt
                        Pz, rPz = bank(C, 2 + (acnt % 2))
                        ab = acnt % 2
                        acnt += 1
                        S.op("pe", lambda e, Pz=Pz, h2=h2, kt=kt, ts=ts: e.matmul(Pz[:], lhsT=kT[:, h2, kt * 128:(kt + 1) * 128], rhs=qT[:, h2, ts], start=True, stop=True),
                             reads=[rk_[h2][kt // 4], rq_[h2][qb]], writes=[rPz])
                        return Pz, rPz, ab

                    nxt = r_qk(0)
                    for kt in range(nk):
                        r = kt - 4 * qb
                        Pz, rPz, ab = nxt
                        if r < 0:
                            S.op("dve", lambda e, Pz=Pz, ab=ab, h2=h2, r=r: e.scalar_tensor_tensor(
                                out=AT[:, ab, :], in0=Pz[:], scalar=gpow[:, h2, -r:-r + 1], in1=base4[:, h2, :], op0=ALU.mult, op1=ALU.mult),
                                reads=[rPz, rtab, C.rcst], writes=[rAT[ab]])
                        else:
                            S.op("dve", lambda e, Pz=Pz, ab=ab, h2=h2, r=r: e.tensor_tensor(out=AT[:, ab, :], in0=Pz[:], in1=diag[:, h2, r, :], op=ALU.mult),
                                 reads=[rPz, rtab], writes=[rAT[ab]])
                        if kt + 1 < nk:
                            nxt = r_qk(kt + 1)
                        S.op("pe", lambda e, Po=Po, kt=kt, h2=h2, ab=ab, nk=nk: e.matmul(Po[:], lhsT=v_tok[:, kt, h2, :], rhs=AT[:, ab, :], start=(kt == 0), stop=(kt == nk - 1)),
                             reads=[rvt[h2][kt], rAT[ab]], writes=[rPo])
                    b = ncnt % 2
                    ncnt += 1
                    S.op("act", lambda e, Po=Po, b=b: e.activation(out=sq[:, b, :], in_=Po[:], func=AF.Square), reads=[rPo], writes=[rsq[b]])
                    Pm, rPm = bank(C, 6 + b)
                    S.op("pe", lambda e, Pm=Pm, b=b: e.matmul(Pm[:], lhsT=ones128, rhs=sq[:, b, :], start=True, stop=True), reads=[rsq[b], C.rcst], writes=[rPm])
                    rstd_from_ps(C, Pm[:], rPm, rs_[:, b, :], rrs[b])
                    S.op("dve", lambda e, Po=Po, b=b: e.tensor_tensor(out=rs_[:, b, :], in0=Po[:], in1=rs_[:, b, :], op=ALU.mult), reads=[rPo, rrs[b]], writes=[rrs[b]])
                    S.op("dve", lambda e, b=b, h2=h2, ts=ts: e.tensor_tensor(out=ost[:, b, :], in0=rs_[:, b, :], in1=sgT[:, h2, ts], op=ALU.mult),
                         reads=[rrs[b], rsg_[h2][qb]], writes=[rost[b]])
                    o_my, romy = out_d
                    r0 = ((qb // 4) * 4 + h2) * 128
                    c0_ = (qb % 4) * 512
                    S.dma("pool", "dma:retout%d" % b, lambda e, b=b, r0=r0, c0_=c0_: e.dma_start(out=o_my[r0:r0 + 128, c0_:c0_ + 512], in_=ost[:, b, :]),
                          reads=[rost[b]], writes=[romy])
            S.barrier_all()
            S.emit()


def phase_att(C, hfull_d, watt_d, bias5_d, out_d, half):
    S = C.S
    with ExitStack() as es:
        aqT = sbt(C, es, "aqT", [64, 2, SEQ], BF16)
        akT = sbt(C, es, "akT", [64, 2, SEQ], BF16)
        av_tok = sbt(C, es, "avtok", [128, NTF, 128], BF16)
        raq = [[Res() for _ in range(8)] for _ in range(2)]
        rak = [[Res() for _ in range(8)] for _ in range(2)]
        rav = [Res() for _ in range(NTF)]
        ones64 = C.cb[0:64, C.CB_ONES64:C.CB_ONES64 + 64]
        with ExitStack() as e1:
            hblk = sbt(C, e1, "hblk", [128, 2, 4096], BF16)
            rhblk = [Res(), Res()]
            hcnt = [0]
            sq = sbt(C, e1, "asq", [64, 2, 512], BF16)
            rsq = [Res(), Res()]
            rs_ = sbt(C, e1, "ars", [64, 2, 512], F32)
            rrs = [Res(), Res()]
            wv = watt_d.rearrange("(kt p) n -> p kt n", p=128)
            wC, rwC = load_w(C, wv[:, :, 0:256], 8, 256)
            wD, rwD = load_w(C, wv[:, :, 256:384], 8, 128)
            cnt = 0
            for tb in range(8):
                hb, rhb = load_hblk(C, hfull_d, tb, hblk, rhblk, hcnt)
                ts = slice(tb * 512, (tb + 1) * 512)
                ulist = [(which, a) for which in range(2) for a in range(2)]
                ust = {}

                def p1(ui, hb=hb, rhb=rhb):
                    nonlocal cnt
                    which, a = ulist[ui]
                    b = cnt % 2
                    cnt += 1
                    Pq, rPq = bank(C, b)
                    proj_fm(C, wC, rwC, which * 128 + a * 64, 64, hb, rhb, Pq, rPq)
                    S.op("act", lambda e, Pq=Pq, b=b: e.activation(out=sq[:, b, :], in_=Pq[0:64, :], func=AF.Square), reads=[rPq], writes=[rsq[b]])
                    ust[ui] = (Pq, rPq, b)

                def p2(ui, ts=ts, tb=tb):
                    which, a = ulist[ui]
                    Pq, rPq, b = ust[ui]
                    Pm, rPm = bank(C, 2 + b)
                    S.op("pe", lambda e, Pm=Pm, b=b: e.matmul(Pm[0:64, :], lhsT=ones64, rhs=sq[:, b, :], start=True, stop=True),
                         reads=[rsq[b], C.rcst], writes=[rPm])
                    S.op("act", lambda e, Pm=Pm, b=b: e.activation(out=rs_[:, b, :], in_=Pm[0:64, :], func=AF.Ln, bias=C.cf[0:64, C.CF_EPS:C.CF_EPS + 1]),
                         reads=[rPm, C.rcst], writes=[rrs[b]])
                    S.op("act", lambda e, b=b: e.activation(out=rs_[:, b, :], in_=rs_[:, b, :], func=AF.Exp, scale=-0.5), reads=[rrs[b]], writes=[rrs[b]])
                    dst = aqT if which == 0 else akT
                    rd = raq if which == 0 else rak
                    gcol = C.cf[0:64, C.CF_QG + which:C.CF_QG + which + 1]
                    S.op("dve", lambda e, Pq=Pq, b=b, dst=dst, a=a, ts=ts, gcol=gcol: e.scalar_tensor_tensor(
                        out=dst[:, a, ts], in0=Pq[0:64, :], scalar=gcol, in1=rs_[:, b, :], op0=ALU.mult, op1=ALU.mult),
                        reads=[rPq, rrs[b], C.rcst], writes=[rd[a][tb]])

                p1(0)
                for ui in range(len(ulist)):
                    if ui + 1 < len(ulist):
                        p1(ui + 1)
                    p2(ui)
                for j in range(4):
                    Tt = tb * 4 + j
                    Pt, rPt = bank(C, 4 + (j % 2))
                    proj_tok(C, wD, rwD, 0, 128, hb, rhb, j, Pt, rPt)
                    S.op("act", lambda e, Tt=Tt, Pt=Pt: e.copy(out=av_tok[:, Tt, :], in_=Pt[:, 0:128]), reads=[rPt], writes=[rav[Tt]])
            S.barrier_all()
            S.emit()
        with ExitStack() as e2:
            bias5 = sbt(C, e2, "bias5", [128, 2, 640], F32)
            rb5 = Res()
            S.dma("sp", "dma:bias5", lambda e: e.dma_start(out=bias5[:], in_=bias5_d), writes=[rb5])
            tmp = sbt(C, e2, "atmp", [128, 2, 640], F32)
            rtmp = [Res(), Res()]
            PT = sbt(C, e2, "aPT", [128, 2, 640], BF16)
            rPT = [Res(), Res()]
            rden = sbt(C, e2, "rden", [64, 2, 512], F32)
            rrd = [Res(), Res()]
            ost = sbt(C, e2, "aost", [64, 2, 512], BF16)
            rost = [Res(), Res()]
            one64 = C.cb[:, C.CB_ONE64:C.CB_ONE64 + 64]
            units = [(qb, a, jq) for qb in range(8) for a in range(2) for jq in range(4)]
            uinfo = {}

            def a_qk(ui):
                qb, a, jq = units[ui]
                qt = qb * 4 + jq
                b = ui % 2
                ZA, rZA = bank(C, b)
                ZB, rZB = bank(C, 2 + b)
                kmin = max(0, 4 - qt)
                qs = slice(qt * 128, (qt + 1) * 128)
                for kr in range(kmin, 5):
                    kt = qt - 4 + kr
                    Z, rZ, zo = (ZA, rZA, kr * 128) if kr < 4 else (ZB, rZB, 0)
                    S.op("pe", lambda e, Z=Z, zo=zo, a=a, kt=kt, qs=qs: e.matmul(
                        Z[:, zo:zo + 128], lhsT=akT[:, a, kt * 128:(kt + 1) * 128], rhs=aqT[:, a, qs], start=True, stop=True),
                        reads=[rak[a][kt // 4], raq[a][qb]], writes=[rZ])
                uinfo[ui] = (ZA, rZA, ZB, rZB, kmin, b, qt)

            def a_ew(ui):
                qb, a, jq = units[ui]
                ZA, rZA, ZB, rZB, kmin, b, qt = uinfo[ui]
                if kmin < 4:
                    S.op("dve", lambda e, ZA=ZA, b=b, a=a, kmin=kmin: e.scalar_tensor_tensor(
                        out=tmp[:, b, kmin * 128:512], in0=ZA[:, kmin * 128:512], scalar=0.125, in1=bias5[:, a, kmin * 128:512],
                        op0=ALU.mult, op1=ALU.add), reads=[rZA, rb5], writes=[rtmp[b]])
                S.op("dve", lambda e, ZB=ZB, b=b, a=a: e.scalar_tensor_tensor(
                    out=tmp[:, b, 512:640], in0=ZB[:, 0:128], scalar=0.125, in1=bias5[:, a, 512:640], op0=ALU.mult, op1=ALU.add),
                    reads=[rZB, rb5], writes=[rtmp[b]])
                S.op("act", lambda e, b=b, kmin=kmin: e.activation(out=PT[:, b, kmin * 128:640], in_=tmp[:, b, kmin * 128:640], func=AF.Exp),
                     reads=[rtmp[b]], writes=[rPT[b]])

            def a_pv(ui):
                qb, a, jq = units[ui]
                ZA, rZA, ZB, rZB, kmin, b, qt = uinfo[ui]
                ob = (qb * 2 + a) % 2
                Po, rPo = bank(C, 4 + ob)
                Pd, rPd = bank(C, 6 + ob)
                for kr in range(kmin, 5):
                    kt = qt - 4 + kr
                    S.op("pe", lambda e, Po=Po, jq=jq, kt=kt, a=a, b=b, kr=kr, kmin=kmin: e.matmul(
                        Po[0:64, jq * 128:(jq + 1) * 128], lhsT=av_tok[:, kt, a * 64:(a + 1) * 64], rhs=PT[:, b, kr * 128:(kr + 1) * 128],
                        start=(kr == kmin), stop=(kr == 4)), reads=[rav[kt], rPT[b]], writes=[rPo])
                    S.op("pe", lambda e, Pd=Pd, jq=jq, b=b, kr=kr, kmin=kmin: e.matmul(
                        Pd[0:64, jq * 128:(jq + 1) * 128], lhsT=one64, rhs=PT[:, b, kr * 128:(kr + 1) * 128],
                        start=(kr == kmin), stop=(kr == 4)), reads=[C.rcst, rPT[b]], writes=[rPd])
                if jq == 3:
                    ts = slice(qb * 512, (qb + 1) * 512)
                    S.op("dve", lambda e, Pd=Pd, ob=ob: e.reciprocal(out=rden[:, ob, :], in_=Pd[0:64, :]), reads=[rPd], writes=[rrd[ob]])
                    S.op("dve", lambda e, Po=Po, ob=ob: e.tensor_tensor(out=ost[:, ob, :], in0=Po[0:64, :], in1=rden[:, ob, :], op=ALU.mult),
                         reads=[rPo, rrd[ob]], writes=[rost[ob]])
                    o_my, romy = out_d
                    r0 = ((qb // 4) * 4 + 2 + half) * 128 + a * 64
                    c0_ = (qb % 4) * 512
                    S.dma("sp", "dma:attout%d" % ob, lambda e, ob=ob, r0=r0, c0_=c0_: e.dma_start(out=o_my[r0:r0 + 64, c0_:c0_ + 512], in_=ost[:, ob, :]),
                          reads=[rost[ob]], writes=[romy])

            a_qk(0)
            for ui in range(len(units)):
                a_ew(ui)
                if ui + 1 < len(units):
                    a_qk(ui + 1)
                a_pv(ui)
            S.barrier_all()
            S.emit()


def phase_sb(C, hfull_d, wsb_d, m01_d, out_d):
    S = C.S
    trineg = C.cb[:, C.CB_TRIN:C.CB_TRIN + 128]
    omtneg = C.cb[:, C.CB_OMTN:C.CB_OMTN + 128]
    onecol = C.cf[:, C.CF_ONE:C.CF_ONE + 1]
    with ExitStack() as es:
        m01 = sbt(C, es, "m01", [128, 2, 1024], BF16)
        rm01 = Res()
        S.dma("pool", "dma:m01", lambda e: e.dma_start(out=m01[:], in_=m01_d.rearrange("r p n -> p r n")), writes=[rm01])
        for grp in range(4):
            with ExitStack() as eg:
                qT = sbt(C, eg, "sqT", [64, 2, SEQ], BF16)
                kT = sbt(C, eg, "skT", [64, 2, SEQ], BF16)
                v_tok = sbt(C, eg, "svtok", [128, NTF, 128], BF16)
                rq = [[Res() for _ in range(8)] for _ in range(2)]
                rk = [[Res() for _ in range(8)] for _ in range(2)]
                rv = [Res() for _ in range(NTF)]
                with ExitStack() as e1:
                    hblk = sbt(C, e1, "hblk", [128, 2, 4096], BF16)
                    rhblk = [Res(), Res()]
                    hcnt = [0]
                    wv = wsb_d[grp].rearrange("(kt p) n -> p kt n", p=128)
                    wC, rwC = load_w(C, wv[:, :, 0:256], 8, 256)
                    wD, rwD = load_w(C, wv[:, :, 256:384], 8, 128)
                    cnt = 0
                    for tb in range(8):
                        hb, rhb = load_hblk(C, hfull_d, tb, hblk, rhblk, hcnt)
                        ts = slice(tb * 512, (tb + 1) * 512)
                        for which in range(2):
                            for a in range(2):
                                b = cnt % 4
                                cnt += 1
                                Pq, rPq = bank(C, b)
                                proj_fm(C, wC, rwC, which * 128 + a * 64, 64, hb, rhb, Pq, rPq)
                                dst = qT if which == 0 else kT
                                rd = rq if which == 0 else rk
                                if cnt % 2:
                                    S.op("act", lambda e, Pq=Pq, dst=dst, a=a, ts=ts: e.copy(out=dst[:, a, ts], in_=Pq[0:64, :]), reads=[rPq], writes=[rd[a][tb]])
                                else:
                                    S.op("dve", lambda e, Pq=Pq, dst=dst, a=a, ts=ts: e.tensor_copy(out=dst[:, a, ts], in_=Pq[0:64, :]), reads=[rPq], writes=[rd[a][tb]])
                        for j in range(4):
                            Tt = tb * 4 + j
                            Pt, rPt = bank(C, 4 + (j % 2))
                            proj_tok(C, wD, rwD, 0, 128, hb, rhb, j, Pt, rPt)
                            S.op("act", lambda e, Tt=Tt, Pt=Pt: e.copy(out=v_tok[:, Tt, :], in_=Pt[:, 0:128]), reads=[rPt], writes=[rv[Tt]])
                    S.barrier_all()
                    S.emit()
                with ExitStack() as e2:
                    NB = 3
                    eb = sbt(C, e2, "seb", [128, NB, 1024], BF16)
                    spb = sbt(C, e2, "sspb", [128, 2, 1024], BF16)
                    ecb = sbt(C, e2, "secb", [128, 2, 1024], BF16)
                    ab = sbt(C, e2, "sab", [128, 2, 1024], BF16)
                    reb = [Res() for _ in range(NB)]
                    rspb = [Res(), Res()]
                    recb = [Res(), Res()]
                    rab = [Res(), Res()]
                    ost = sbt(C, e2, "sost", [64, 2, 512], BF16)
                    rost = [Res(), Res()]
                    onesneg = C.cb[:, C.CB_ONESN:C.CB_ONESN + 128]
                    XX = C.PP[2]
                    XA, rXA = bank(C, 4)
                    XB, rXB = bank(C, 5)
                    zc = 0
                    ec = 0
                    pc = 0
                    oc = 0
                    for a in range(2):
                        for qb in range(8):
                            ts = slice(qb * 512, (qb + 1) * 512)
                            nk = 4 * qb + 4
                            npair = nk // 2
                            ob = oc % 2
                            oc += 1
                            O, rO = bank(C, 6 + ob)

                            zinfo = {}

                            def qk(pi):
                                nonlocal zc
                                k1 = nk - 1 - 2 * pi
                                zb = zc % 2
                                zc += 1
                                ZZ = C.PP[zb]
                                rZ = [C.rP[2 * zb], C.rP[2 * zb + 1]]
                                for hh_, kt in enumerate((k1, k1 - 1)):
                                    S.op("pe", lambda e, ZZ=ZZ, hh_=hh_, kt=kt, a=a, ts=ts: e.matmul(
                                        ZZ[:, hh_ * 512:(hh_ + 1) * 512], lhsT=kT[:, a, kt * 128:(kt + 1) * 128], rhs=qT[:, a, ts], start=True, stop=True),
                                        reads=[rk[a][kt // 4], rq[a][qb]], writes=[rZ[hh_]])
                                zinfo[pi] = (ZZ, rZ)

                            def e_op(pi):
                                nonlocal ec
                                ZZ, rZ = zinfo[pi]
                                ei = ec % NB
                                ec += 1
                                S.op("act", lambda e, ZZ=ZZ, ei=ei: e.activation(out=eb[:, ei, :], in_=ZZ[:], func=AF.Exp, scale=0.125), reads=rZ, writes=[reb[ei]])
                                return ei

                            def av(pi, pb):
                                k1 = nk - 1 - 2 * pi
                                for hh_, kt in enumerate((k1, k1 - 1)):
                                    S.op("pe", lambda e, O=O, kt=kt, pb=pb, hh_=hh_, pi=pi, a=a, npair=npair: e.matmul(
                                        O[0:64, :], lhsT=v_tok[:, kt, a * 64:(a + 1) * 64], rhs=ab[:, pb, hh_ * 512:(hh_ + 1) * 512],
                                        start=(pi == 0 and hh_ == 0), stop=(pi == npair - 1 and hh_ == 1)),
                                        reads=[rv[kt], rab[pb]], writes=[rO])

                            qk(0)
                            eis = {0: e_op(0)}
                            if npair > 1:
                                qk(1)
                            pbs = {}
                            for pi in range(npair):
                                k1 = nk - 1 - 2 * pi
                                k0 = k1 - 1
                                ei = eis[pi]
                                pb = pc % 2
                                pc += 1
                                pbs[pi] = pb
                                if k0 >= 4 * qb:
                                    mp = 0 if (k1 - 4 * qb) == 3 else 1
                                    S.op("dve", lambda e, ei=ei, mp=mp: e.tensor_tensor(out=eb[:, ei, :], in0=eb[:, ei, :], in1=m01[:, mp, :], op=ALU.mult),
                                         reads=[reb[ei], rm01], writes=[reb[ei]])
                                S.op("act", lambda e, ei=ei, pb=pb: e.activation(out=spb[:, pb, :], in_=eb[:, ei, :], func=AF.Ln, bias=onecol),
                                     reads=[reb[ei], C.rcst], writes=[rspb[pb]])
                                if pi + 1 < npair:
                                    eis[pi + 1] = e_op(pi + 1)
                                first = (pi == 0)
                                S.op("pe", lambda e, pb=pb, first=first: e.matmul(XA[:], lhsT=trineg, rhs=spb[:, pb, 0:512], start=first, stop=True),
                                     reads=[rspb[pb], C.rcst], writes=[rXA])
                                S.op("pe", lambda e, pb=pb, first=first: e.matmul(XB[:], lhsT=onesneg, rhs=spb[:, pb, 0:512], start=first, stop=True),
                                     reads=[rspb[pb], C.rcst], writes=[rXB])
                                S.op("pe", lambda e, pb=pb: e.matmul(XB[:], lhsT=trineg, rhs=spb[:, pb, 512:1024], start=False, stop=True),
                                     reads=[rspb[pb], C.rcst], writes=[rXB])
                                if pi >= 1:
                                    av(pi - 1, pbs[pi - 1])
                                if pi + 2 < npair:
                                    qk(pi + 2)
                                S.op("act", lambda e, pb=pb: e.activation(out=ecb[:, pb, :], in_=XX[:], func=AF.Exp), reads=[rXA, rXB], writes=[recb[pb]])
                                if pi + 1 < npair:
                                    S.op("pe", lambda e, pb=pb: e.matmul(XA[:], lhsT=omtneg, rhs=spb[:, pb, 0:512], start=False, stop=True),
                                         reads=[rspb[pb], C.rcst], writes=[rXA])
                                    S.op("pe", lambda e, pb=pb: e.matmul(XA[:], lhsT=onesneg, rhs=spb[:, pb, 512:1024], start=False, stop=True),
                                         reads=[rspb[pb], C.rcst], writes=[rXA])
                                    S.op("pe", lambda e, pb=pb: e.matmul(XB[:], lhsT=omtneg, rhs=spb[:, pb, 512:1024], start=False, stop=True),
                                         reads=[rspb[pb], C.rcst], writes=[rXB])
                                S.op("dve", lambda e, ei=ei, pb=pb: e.tensor_tensor(out=ab[:, pb, :], in0=eb[:, ei, :], in1=ecb[:, pb, :], op=ALU.mult),
                                     reads=[reb[ei], recb[pb]], writes=[rab[pb]])
                            av(npair - 1, pbs[npair - 1])
                            S.op("dve", lambda e, O=O, ob=ob: e.tensor_copy(out=ost[:, ob, :], in_=O[0:64, :]), reads=[rO], writes=[rost[ob]])
                            o_my, romy = out_d
                            r0 = ((qb // 4) * 4 + grp) * 128 + a * 64
                            c0_ = (qb % 4) * 512
                            S.dma("sp", "dma:sbout%d" % ob, lambda e, ob=ob, r0=r0, c0_=c0_: e.dma_start(out=o_my[r0:r0 + 64, c0_:c0_ + 512], in_=ost[:, ob, :]),
                                  reads=[rost[ob]], writes=[romy])
                    S.barrier_all()
                    S.emit()


def phase_h_out(C, hT, rh, out_d):
    S = C.S
    h_my, rhmy = out_d
    for tb in range(4):
        ts = slice(tb * 512, (tb + 1) * 512)
        S.dma("sp", "dma:hout", lambda e, tb=tb, ts=ts: e.dma_start(
            out=h_my[tb * 128:(tb + 1) * 128, :].rearrange("p (k n) -> p k n", k=8), in_=hT[:, :, ts]),
            reads=[rh[dt][tb] for dt in range(8)], writes=[rhmy])
    S.barrier_all()
    S.emit()


def phase_load_mix(C, mix_d, hT, rh, l):
    S = C.S
    o_all, roall = mix_d
    with ExitStack() as es:
        st = sbt(C, es, "mixst", [128, 2, 2, T], BF16)
        rst = [[Res(), Res()], [Res(), Res()]]
        tm = sbt(C, es, "mixtm", [128, 2, 2, T], BF16)
        rtm = [[Res(), Res()], [Res(), Res()]]
        for kt in range(8):
            if l == 0:
                r, fb = (kt // 2, kt % 2) if kt < 4 else ((kt - 4) // 2, 2 + (kt - 4) % 2)
            else:
                r, fb = kt // 4, kt % 4
            bb = kt % 2
            for sh in range(2):
                r0 = ((sh * 2 + r) * 4 + fb) * 128
                S.dma("sp", "dma:mix%d%d" % (bb, sh), lambda e, bb=bb, sh=sh, r0=r0: e.dma_start(out=st[:, bb, sh, :], in_=o_all[r0:r0 + 128, :]),
                      reads=[roall], writes=[rst[bb][sh]])
                S.op("act", lambda e, bb=bb, sh=sh: e.activation(out=tm[:, bb, sh, :], in_=st[:, bb, sh, :], func=AF.Identity,
                                                                 scale=C.vec[:, C.V_SEL + sh:C.V_SEL + sh + 1]),
                     reads=[rst[bb][sh], C.rvec], writes=[rtm[bb][sh]])
            S.op("dve", lambda e, bb=bb, kt=kt: e.tensor_tensor(out=hT[:, kt, :], in0=tm[:, bb, 0, :], in1=tm[:, bb, 1, :], op=ALU.add),
                 reads=[rtm[bb][0], rtm[bb][1]], writes=[rh[kt][tb] for tb in range(4)])
        S.barrier_all()
        S.emit()


def all_gather(C, src, rsrc, dst, rdst):
    n = src.shape[0] // 2
    for u in range(2):
        C.S.cc("pool", "cc", lambda e, u=u: e.collective_compute(
            "AllGather", ALU.bypass, replica_groups=[[0, 4], [1, 5], [2, 6], [3, 7]],
            ins=[src[u * n:(u + 1) * n, :]], outs=[dst[u * 2 * n:(u + 1) * 2 * n, :]]), reads=[rsrc], writes=[rdst])
    C.S.final_wait("pool", ["cc"])


def phase_wout(C, wout_d, mixT, rmix, xT, rx, g1):
    S = C.S
    wv = wout_d.rearrange("(kt p) n -> p kt n", p=128)
    n = 0
    for half in range(2):
        w, rw = load_w(C, wv[:, :, half * 512:(half + 1) * 512], 8, 512)
        for tb in range(4):
            ts = slice(tb * 512, (tb + 1) * 512)
            for c4 in range(4):
                dt = half * 4 + c4
                P, rP = bank(C, n % 4)
                n += 1
                for kt in range(8):
                    S.op("pe", lambda e, P=P, w=w, kt=kt, c4=c4, ts=ts: e.matmul(
                        P[:], lhsT=w[:, kt, c4 * 128:(c4 + 1) * 128], rhs=mixT[:, kt, ts], start=(kt == 0), stop=(kt == 7)),
                        reads=[rw, rmix[kt][tb]], writes=[rP])
                rxs = [rx[dt][tb * 4 + k] for k in range(4)]
                S.op("dve", lambda e, P=P, dt=dt, ts=ts: e.scalar_tensor_tensor(
                    out=xT[:, dt, ts], in0=P[:], scalar=g1[:, dt:dt + 1], in1=xT[:, dt, ts], op0=ALU.mult, op1=ALU.add),
                    reads=[rP, C.rmod] + rxs, writes=rxs)
    S.barrier_all()
    S.emit()


CF_IDENT, CF_ONE, CF_EPS, CF_QG, CF_GPOW, NCF = 0, 128, 129, 130, 132, 196
CB_IDENT, CB_ONESM, CB_RW, CB_ONES128, CB_ONES64, CB_ONE64, CB_TRIN, CB_OMTN, CB_ONESN, NCB = 0, 128, 256, 384, 512, 576, 640, 768, 896, 1024
V_NG, V_RB, V_ADAB, V_SEL, NV = 0, 32, 48, 144, 146


def set_layout(C):
    C.CF_IDENT, C.CF_ONE, C.CF_EPS, C.CF_QG, C.CF_GPOW = CF_IDENT, CF_ONE, CF_EPS, CF_QG, CF_GPOW
    C.CB_IDENT, C.CB_ONESM, C.CB_RW, C.CB_ONES128, C.CB_ONES64, C.CB_ONE64, C.CB_TRIN, C.CB_OMTN, C.CB_ONESN = (
        CB_IDENT, CB_ONESM, CB_RW, CB_ONES128, CB_ONES64, CB_ONE64, CB_TRIN, CB_OMTN, CB_ONESN)
    C.V_NG, C.V_RB, C.V_ADAB, C.V_SEL = V_NG, V_RB, V_ADAB, V_SEL


def dram_in(nc, name, shape):
    return nc.dram_tensor(name, list(shape), F32, kind="ExternalInput").ap()


def dram_out(nc, name, shape):
    return nc.dram_tensor(name, list(shape), F32, kind="ExternalOutput").ap()


def new_nc():
    return bass.Bass("TRN2", target_bir_lowering=False)


def tok_state(C, es):
    xT = sbt(C, es, "xT", [128, 8, T], F32)
    rx = [[Res() for _ in range(NT)] for _ in range(8)]
    hT = sbt(C, es, "hT", [128, 8, T], BF16)
    rh = [[Res() for _ in range(4)] for _ in range(8)]
    return xT, rx, hT, rh


NIDX = 24


def build_fused(stop=None):
    nc = new_nc()
    I32 = mybir.dt.int32
    x_d = dram_in(nc, "x_own", [T, D]); cvec_d = dram_in(nc, "cvec", [128, 8]); ada_w_d = dram_in(nc, "ada_w", [2, 1024, 6144])
    vecs_d = dram_in(nc, "vecs", [128, NV]); cstb_d = dram_in(nc, "cstb", [128, NCB]); cstf_d = dram_in(nc, "cstf", [128, NCF])
    wret_d = dram_in(nc, "wret", [2, 1024, 768]); watt_d = dram_in(nc, "watt", [2, 1024, 384]); bias5_d = dram_in(nc, "bias5", [2, 128, 2, 640])
    tab = {k: dram_in(nc, k, shp) for k, shp in (("cosT", [128, SEQ]), ("sinT", [128, SEQ]), ("base4", [2, 128, 512]), ("diag", [2, 4, 128, 512]))}
    m01_d = dram_in(nc, "m01", [2, 128, 1024]); wsb_d = dram_in(nc, "wsb", [4, 1024, 384])
    wout_d = [dram_in(nc, "wout%d" % l, [1024, 1024]) for l in range(2)]
    wg_d = [dram_in(nc, "wg%d" % l, [16, 1024, 512]) for l in range(2)]
    wu_d = [dram_in(nc, "wu%d" % l, [16, 1024, 512]) for l in range(2)]
    wd_d = [dram_in(nc, "wd%d" % l, [16, 512, 1024]) for l in range(2)]
    out_d = dram_out(nc, "x_out", [T, D])
    h_my = nc.dram_tensor("h_my", [512, 4096], BF16).ap()
    h_all = nc.dram_tensor("h_all", [2 * 512, 4096], BF16).ap()
    o_my = nc.dram_tensor("o_my", [1024, 2048], BF16).ap()
    o_all = nc.dram_tensor("o_all", [2 * 1024, 2048], BF16).ap()
    rhmy, rhall, romy, roall = Res("h_my"), Res("h_all"), Res("o_my"), Res("o_all")
    with ExitStack() as es:
        C = mk_ctx(nc, es); set_layout(C)
        setup_consts(C, cstb_d, cstf_d)
        xT = sbt(C, es, "xT", [128, 8, T], F32)
        rx = [[Res() for _ in range(NT)] for _ in range(8)]
        alloc_adaln(C, NV)

        def tok_bufs(ph):
            hT = sbt(C, ph, "hT", [128, 8, T], BF16)
            rh = [[Res() for _ in range(4)] for _ in range(8)]
            comb = sbt(C, ph, "comb", [128, 16, 16], BF16)
            ring2 = sbt(C, ph, "ring2", [128, 2, 4096], BF16)
            del C.slots[NSLOT:]
            for i in range(2):
                C.slots.append((ring2[:, i, :], Res("ringx%d" % i), "dma:ringx%d" % i))
            return hT, rh, comb, Res()

        with ExitStack() as ph:
            hT, rh, comb, rcomb = tok_bufs(ph)
            phase_load_x(C, x_d, xT, rx, "xs")
            phase_adaln(C, cvec_d, ada_w_d, vecs_d)
            phase_norm(C, xT, rx, C.Gt[:, 0, 0, :], mod_col(C, 0, 0), hT, rh)
            phase_h_out(C, hT, rh, (h_my, rhmy))
            del C.slots[NSLOT:]
        for l in range(2):
            if stop == 0:
                phase_store_x(C, xT, rx, out_d)
                return nc
            all_gather(C, h_my, rhmy, h_all, rhall)
            if stop == 1:
                phase_store_x(C, xT, rx, out_d)
                return nc
            if stop in (11, 12):
                with ExitStack() as tt:
                    hblk = sbt(C, tt, "hblk", [128, 2, 4096], BF16)
                    rhblk = [Res(), Res()]
                    if stop == 11:
                        import os
                        var = os.environ.get("MK_VAR", "a")
                        if var == "a":
                            for tb_ in (5, 5, 5):
                                hb, rhb = load_hblk(C, (h_all, rhall), tb_, hblk, rhblk, [0])
                        elif var == "b":
                            hb, rhb = load_hblk(C, (h_all, rhall), 5, hblk, rhblk, [1])
                        elif var.startswith("t"):
                            hb, rhb = load_hblk(C, (h_all, rhall), int(var[1:]), hblk, rhblk, [0])
                    else:
                        C.S.dma("pool", "dma:hblk0", lambda e: e.indirect_dma_start(out=hblk[:, 0, 0:2048], out_offset=None, in_=h_all[:, 0:2048],
                                in_offset=bass.IndirectOffsetOnAxis(ap=C.idx[:, 5:6], axis=0)), reads=[rhall, C.ridx], writes=[rhblk[0]])
                    C.S.barrier_all()
                    C.S.emit()
                phase_store_x(C, xT, rx, out_d)
                return nc
            if l == 0:
                phase_ret(C, (h_all, rhall), wret_d, tab, (o_my, romy))
                if stop == 2:
                    phase_store_x(C, xT, rx, out_d)
                    return nc
                for half in range(2):
                    phase_att(C, (h_all, rhall), watt_d[half], bias5_d[half], (o_my, romy), half)
            else:
                phase_sb(C, (h_all, rhall), wsb_d, m01_d, (o_my, romy))
            if stop == 3:
                phase_store_x(C, xT, rx, out_d)
                return nc
            all_gather(C, o_my, romy, o_all, roall)
            if stop == 4:
                phase_store_x(C, xT, rx, out_d)
                return nc
            if stop == 5 + 10 * l:
                dbg_h = nc.dram_tensor("dbg_h", [2 * 512, 4096], BF16, kind="ExternalOutput").ap()
                dbg_o = nc.dram_tensor("dbg_o", [2 * 1024, 2048], BF16, kind="ExternalOutput").ap()
                C.S.dma("sp", "dma:dbg", lambda e: e.dma_start(out=dbg_h, in_=h_all), reads=[rhall])
                C.S.dma("sp", "dma:dbg", lambda e: e.dma_start(out=dbg_o, in_=o_all), reads=[roall])
                C.S.final_wait("sp", ["dma:dbg"])
                phase_store_x(C, xT, rx, out_d)
                return nc
            with ExitStack() as ph:
                hT, rh, comb, rcomb = tok_bufs(ph)
                phase_load_mix(C, (o_all, roall), hT, rh, l)
                phase_wout(C, wout_d[l], hT, rh, xT, rx, mod_col(C, l, 2))
                if stop == 6 and l == 0:
                    phase_store_x(C, xT, rx, out_d)
                    return nc
                phase_norm(C, xT, rx, C.Gt[:, l, 1, :], mod_col(C, l, 3), hT, rh)
                phase_router(C, hT, rh, comb, rcomb)
                phase_moe(C, l, wg_d[l], wu_d[l], wd_d[l], hT, rh, xT, rx, comb, rcomb, mod_col(C, l, 5))
                if stop == 7 and l == 0:
                    phase_store_x(C, xT, rx, out_d)
                    return nc
                if l == 0:
                    phase_norm(C, xT, rx, C.Gt[:, 1, 0, :], mod_col(C, 1, 0), hT, rh)
                    phase_h_out(C, hT, rh, (h_my, rhmy))
                else:
                    phase_store_x(C, xT, rx, out_d)
                del C.slots[NSLOT:]
    return nc


def rope_tables():
    inv = (10000.0 ** (-np.arange(0, 128, 2, dtype=np.float32) / 128)).astype(np.float32)
    pos = np.arange(4096, dtype=np.float32)
    ang = (pos[:, None] * inv[None, :]).astype(np.float32)
    cos = np.cos(ang).astype(np.float32)
    sin = np.sin(ang).astype(np.float32)
    cosT = np.concatenate([cos.T, cos.T], 0)
    sinT = np.concatenate([-sin.T, sin.T], 0)
    cos_tok = cos.reshape(32, 128, 64).transpose(1, 0, 2)
    sin_tok = sin.reshape(32, 128, 64).transpose(1, 0, 2)
    return dict(cosT=np.ascontiguousarray(cosT), sinT=np.ascontiguousarray(sinT),
                cos_tok=np.ascontiguousarray(cos_tok), sin_tok=np.ascontiguousarray(sin_tok))

def ret_consts(heads):
    qdec = np.zeros((128, 2, 64), np.float32)
    kdec = np.zeros((128, 2), np.float32)
    cdec = np.zeros((128, 2), np.float32)
    decp = np.zeros((128, 2, 128), np.float32)
    i = np.arange(64, dtype=np.float64)
    p = np.arange(128)
    for n, h in enumerate(heads):
        g = 1.0 - 2.0 ** (-5.0 - h)
        lg = np.log(g)
        qdec[:, n, :] = np.exp(lg * (i + 1))[None, :]
        kdec[:, n] = np.exp(lg * (63 - (p % 64))) * (128 ** -0.5)
        cdec[:, n] = np.exp(lg * 64)
        jj = p[:, None]; ii = p[None, :]
        same = (jj // 64) == (ii // 64)
        e = np.abs((ii % 64) - (jj % 64)) - (ii % 64) - 1
        decp[:, n, :] = np.where(same, np.exp(lg * e) * (128 ** -0.5), 0.0)
    return qdec.reshape(128, 128), kdec, cdec, decp.reshape(128, 256)

def wret_for(w_in, heads):
    out = np.zeros((2, 1024, 768), np.float32)
    sw = np.concatenate([np.arange(64, 128), np.arange(0, 64)])
    for n, h in enumerate(heads):
        rq = w_in[:, h * 128:(h + 1) * 128]
        rk = w_in[:, 512 + h * 128:512 + (h + 1) * 128]
        rv = w_in[:, 1024 + h * 128:1024 + (h + 1) * 128]
        rg = w_in[:, 1536 + h * 128:1536 + (h + 1) * 128]
        out[n] = np.concatenate([rq, rq[:, sw], rk, rk[:, sw], rg, rv], 1)
    return out

def ret_tables(heads):
    base4 = np.zeros((2, 128, 512), np.float32)
    diag = np.zeros((2, 4, 128, 512), np.float32)
    gpow = np.zeros((128, 2, 32), np.float32)
    j = np.arange(128, dtype=np.float64)[:, None]
    i = np.arange(512, dtype=np.float64)[None, :]
    sc = 128 ** -0.5
    for n, h in enumerate(heads):
        g = 1.0 - 2.0 ** (-5.0 - h)
        lg = np.log(g)
        base4[n] = np.exp(lg * (i - j)) * sc
        for m in range(32):
            gpow[:, n, m] = np.exp(lg * 128.0 * m)
        for r in range(4):
            sj = 128 * r + j
            cs_, ct = np.floor(sj / 64), np.floor(i / 64)
            d = np.where(cs_ > ct, 0.0, np.where(cs_ == ct, np.exp(lg * np.abs(i - sj)), np.exp(lg * np.maximum(i - sj, 0))))
            diag[n, r] = d * sc
    return base4, diag, gpow

NEG = -30000.0
def att_bias5(rel_bias, heads):
    j = np.arange(128)[:, None, None]
    kr = np.arange(5)[None, :, None]
    i = np.arange(128)[None, None, :]
    kk = 128 * kr + j
    dist = 512 + i - kk
    idx = np.clip(dist, -63, 128) + 63
    cq = 8 + i // 64
    ck = kk // 64
    valid = (ck <= cq) & (ck >= cq - 8)
    out = np.zeros((128, len(heads), 5, 128), np.float32)
    for n, h in enumerate(heads):
        g = rel_bias[h][idx]
        out[:, n] = np.where(valid, g, np.float32(NEG))
    return out.reshape(128, len(heads), 640)

def watt_for(w_in, heads):
    cols = []
    for base in (2048, 2560, 3072):
        for h in heads:
            cols.append(w_in[:, base + h * 64: base + (h + 1) * 64])
    return np.ascontiguousarray(np.concatenate(cols, 1))

def sb_consts():
    j = np.arange(128)[:, None]
    s = np.arange(128)[None, :]
    trineg = -(j >= s).astype(np.float32)
    omtneg = -(j < s).astype(np.float32)
    i = np.arange(512)[None, None, :]
    r = np.arange(4)[:, None, None]
    jj = np.arange(128)[None, :, None]
    m01 = ((128 * r + jj) < i).astype(np.float32)
    m01p = np.stack([np.concatenate([m01[3], m01[2]], 1), np.concatenate([m01[1], m01[0]], 1)])
    return trineg, omtneg, np.ascontiguousarray(m01p)

def wsb_for(w_in, heads8):
    out = np.zeros((4, 1024, 384), np.float32)
    for g in range(4):
        cols = []
        for base in (0, 1024, 2048):
            for h in heads8[g * 2:(g + 1) * 2]:
                cols.append(w_in[:, base + h * 64: base + (h + 1) * 64])
        out[g] = np.concatenate(cols, 1)
    return out


def idx_table(core):
    b, s = core // 2, core % 2
    p = np.arange(128)
    idx = np.zeros((128, 24), np.int32)
    for tb in range(8):
        rank = 2 * b + tb // 4
        idx[:, tb] = (rank * 4 + tb % 4) * 128 + p
    for kt in range(8):
        if kt < 4:
            rank, fb = 2 * b + kt // 2, kt % 2
        else:
            rank, fb = 2 * b + (kt - 4) // 2, 2 + (kt - 4) % 2
        idx[:, 8 + kt] = ((rank * 2 + s) * 4 + fb) * 128 + p
        rank, fb = 2 * b + kt // 4, kt % 4
        idx[:, 16 + kt] = ((rank * 2 + s) * 4 + fb) * 128 + p
    return idx


def _const_packs(inp, core):
    b, hh = core % 4, core // 4
    cstf = np.zeros((128, NCF), np.float32)
    cstf[:, CF_IDENT:CF_IDENT + 128] = np.eye(128, dtype=np.float32)
    cstf[:, CF_ONE] = 1.0
    cstf[:, CF_EPS] = EPS
    cstf[:, CF_QG] = np.tile(inp["att_q_norm_g"][0], 2)
    cstf[:, CF_QG + 1] = np.tile(inp["att_k_norm_g"][0], 2)
    base4, diag, gpow = ret_tables([2 * hh, 2 * hh + 1])
    cstf[:, CF_GPOW:CF_GPOW + 64] = gpow.reshape(128, 64)
    trineg, omtneg, m01 = sb_consts()
    cstb = np.zeros((128, NCB), np.float32)
    cstb[:, CB_IDENT:CB_IDENT + 128] = np.eye(128, dtype=np.float32)
    cstb[:, CB_ONESM:CB_ONESM + 128] = 1.0 / 1024
    cstb[:, CB_RW:CB_RW + 128] = inp["router_w"].reshape(8, 128, 16).transpose(1, 0, 2).reshape(128, 128)
    cstb[:, CB_ONES128:CB_ONES128 + 128] = 1.0 / 128
    cstb[:, CB_ONES64:CB_ONES64 + 64] = 1.0 / 64
    cstb[:, CB_ONE64:CB_ONE64 + 64] = 1.0
    cstb[:, CB_TRIN:CB_TRIN + 128] = trineg
    cstb[:, CB_OMTN:CB_OMTN + 128] = omtneg
    cstb[:, CB_ONESN:CB_ONESN + 128] = -1.0
    vecs = np.zeros((128, NV), np.float32)
    for l in range(2):
        vecs[:, V_NG + (l * 2 + 0) * 8:V_NG + (l * 2 + 0) * 8 + 8] = inp["norm1_g"][l].reshape(8, 128).T
        vecs[:, V_NG + (l * 2 + 1) * 8:V_NG + (l * 2 + 1) * 8 + 8] = inp["norm2_g"][l].reshape(8, 128).T
        vecs[:, V_ADAB + l * 48:V_ADAB + (l + 1) * 48] = inp["ada_b"][l].reshape(48, 128).T
    vecs[:, V_RB:V_RB + 16] = inp["router_b"][None, :]
    vecs[:, V_SEL + hh] = 1.0
    return dict(cstf=cstf, cstb=cstb, vecs=vecs, base4=base4, diag=diag, m01=m01)


_NC = []
_STOP = None
_LAST = None
_EXEC = None
_RUNKW = {}


def kernel(**inputs):
    inp = {k: np.ascontiguousarray(np.asarray(v, dtype=np.float32)) for k, v in inputs.items()}
    x = inp["x"]
    tabs = rope_tables()
    if not _NC:
        _NC.append(build_fused(_STOP))
    nc = _NC[0]
    w_in0 = inp["even_w_in"][0]
    w_in1 = inp["odd_w_in"][0]
    in_maps = []
    for c in range(8):
        b, i = c % 4, c // 4
        pk = _const_packs(inp, c)
        m = {"x_own": np.ascontiguousarray(x[b, i * T:(i + 1) * T]),
             "cvec": np.ascontiguousarray(inp["c"][b].reshape(8, 128).T),
             "ada_w": inp["ada_w"], "vecs": pk["vecs"], "cstb": pk["cstb"], "cstf": pk["cstf"],
             "wret": wret_for(w_in0, [2 * i, 2 * i + 1]),
             "watt": np.stack([watt_for(w_in0, [4 * i + 2 * hf, 4 * i + 2 * hf + 1]) for hf in range(2)]),
             "bias5": np.stack([att_bias5(inp["att_rel_bias"][0], [4 * i + 2 * hf, 4 * i + 2 * hf + 1]) for hf in range(2)]),
             "cosT": tabs["cosT"], "sinT": tabs["sinT"], "base4": pk["base4"], "diag": pk["diag"], "m01": pk["m01"],
             "wsb": wsb_for(w_in1, [8 * i + j for j in range(8)]),
             "wout0": inp["even_w_out"][0], "wout1": inp["odd_w_out"][0]}
        for l in range(2):
            m["wg%d" % l] = inp["exp_w_gate"][l]
            m["wu%d" % l] = inp["exp_w_up"][l]
            m["wd%d" % l] = inp["exp_w_down"][l]
        in_maps.append(m)
    res = run_bass_kernel_spmd(nc, in_maps, core_ids=list(range(8)), **_RUNKW)
    global _LAST, _EXEC
    _EXEC = res.exec_time_ns
    global _LAST
    _LAST = res.results
    out = np.zeros((4, SEQ, D), np.float32)
    for c in range(8):
        b, i = c % 4, c // 4
        out[b, i * T:(i + 1) * T] = res.results[c]["x_out"]
    return out
```

```python
from concourse.bass_utils import run_bass_kernel_spmd

import math
import numpy as np
import concourse.bass as bass
import concourse.mybir as mybir
from contextlib import ExitStack

F32 = mybir.dt.float32
BF16 = mybir.dt.bfloat16
ALU = mybir.AluOpType
AF = mybir.ActivationFunctionType
AX = mybir.AxisListType

D = 1024
T = 2048
NT = T // 128
NSLOT = 4
EPS = 1e-6
NEG = -30000.0


class Res:
    __slots__ = ("name", "lw", "rd")

    def __init__(self, name=""):
        self.name = name
        self.lw = None
        self.rd = {}


class Sched:
    ENG = ["pe", "act", "dve", "pool", "sp"]

    def __init__(self, nc, es):
        self.nc = nc
        self.es = es
        self.q = {e: [] for e in self.ENG}
        self.cnt = {}
        self.sem = {}
        self.seen = {e: {} for e in self.ENG}
        self.step = {}
        self.out_streams = []
        for e in self.ENG:
            self._mk(e, 1)

    def _mk(self, name, step):
        self.sem[name] = self.es.enter_context(self.nc.semaphore("s_" + name.replace(":", "_")))
        self.cnt[name] = 0
        self.step[name] = step

    def _waits(self, eng, reads, writes):
        deps = {}

        def add(d):
            if d is None:
                return
            f, i = d
            if f == eng and eng == "pe":
                return
            if deps.get(f, 0) < i:
                deps[f] = i

        for r in reads:
            add(r.lw)
        for w in writes:
            add(w.lw)
            for f, i in w.rd.items():
                add((f, i))
        ws = []
        for f, i in deps.items():
            if self.seen[eng].get(f, 0) >= i:
                continue
            self.seen[eng][f] = i
            ws.append((self.sem[f], i * self.step[f]))
        return ws

    def op(self, eng, fn, reads=(), writes=()):
        ws = self._waits(eng, reads, writes)
        self.cnt[eng] += 1
        idx = self.cnt[eng]
        sem = self.sem[eng]
        for r in reads:
            r.rd[eng] = idx
        for w in writes:
            w.lw = (eng, idx)
            w.rd = {}

        def run(e, ws=ws, fn=fn, sem=sem):
            for s, v in ws:
                e.wait_ge(s, v)
            fn(e).then_inc(sem, 1)

        self.q[eng].append(run)

    def dma(self, qeng, stream, fn, reads=(), writes=()):
        if stream not in self.sem:
            self._mk(stream, 16)
        ws = self._waits(qeng, reads, writes)
        self.cnt[stream] += 1
        idx = self.cnt[stream]
        sem = self.sem[stream]
        for r in reads:
            r.rd[stream] = idx
        for w in writes:
            w.lw = (stream, idx)
            w.rd = {}

        def run(e, ws=ws, fn=fn, sem=sem):
            for s, v in ws:
                e.wait_ge(s, v)
            fn(e).then_inc(sem, 16)

        self.q[qeng].append(run)

    def cc(self, qeng, stream, fn, reads=(), writes=()):
        if stream not in self.sem:
            self._mk(stream, 1)
        ws = self._waits(qeng, reads, writes)
        self.cnt[stream] += 1
        idx = self.cnt[stream]
        sem = self.sem[stream]
        for r in reads:
            r.rd[stream] = idx
        for w in writes:
            w.lw = (stream, idx)
            w.rd = {}

        def run(e, ws=ws, fn=fn, sem=sem):
            for s, v in ws:
                e.wait_ge(s, v)
            fn(e).then_inc(sem)

        self.q[qeng].append(run)

    def final_wait(self, eng, streams):
        ws = [(self.sem[s], self.cnt[s] * self.step[s]) for s in streams if s in self.sem]

        def run(e, ws=ws):
            for s, v in ws:
                e.wait_ge(s, v)

        self.q[eng].append(run)

    def barrier_all(self):
        snap = {k: self.cnt[k] for k in self.cnt}
        for eng in self.ENG:
            ws = []
            for f, i in snap.items():
                if f == eng or i == 0:
                    continue
                if self.seen[eng].get(f, 0) >= i:
                    continue
                self.seen[eng][f] = i
                ws.append((self.sem[f], i * self.step[f]))

            def run(e, ws=ws):
                for s, v in ws:
                    e.wait_ge(s, v)

            self.q[eng].append(run)

    def emit(self):
        q = self.q
        self.q = {e: [] for e in self.ENG}
        with self.nc.Block() as block:
            @block.tensor
            def _(e):
                for f in q["pe"]:
                    f(e)

            @block.scalar
            def _(e):
                for f in q["act"]:
                    f(e)

            @block.vector
            def _(e):
                for f in q["dve"]:
                    f(e)

            @block.gpsimd
            def _(e):
                for f in q["pool"]:
                    f(e)

            @block.sync
            def _(e):
                for f in q["sp"]:
                    f(e)


class Ctx:
    pass


def mk_ctx(nc, es):
    C = Ctx()
    C.nc = nc
    C.es = es
    C.S = Sched(nc, es)
    C.uid = 0
    C.P = []
    C.rP = []
    C.PP = []
    for j in range(4):
        pp = es.enter_context(nc.psum_tensor("pbb%d" % j, [128, 1024], F32))
        C.PP.append(pp)
        for h in range(2):
            C.P.append(pp[:, h * 512:(h + 1) * 512])
            C.rP.append(Res("pb%d" % (2 * j + h)))
    C.ring = es.enter_context(nc.sbuf_tensor("ring", [128, NSLOT, 4096], BF16))
    C.slots = [(C.ring[:, i, :], Res("ring%d" % i), "dma:ring%d" % i) for i in range(NSLOT)]
    C.ring_i = 0
    return C


def sbt(C, es, name, shape, dt):
    C.uid += 1
    return es.enter_context(C.nc.sbuf_tensor("%s_%d" % (name, C.uid), list(shape), dt))


def load_w(C, view, kt, ncols):
    assert kt * ncols <= 4096
    i = C.ring_i % len(C.slots)
    C.ring_i += 1
    base, r, stream = C.slots[i]
    dst = base[:, 0:kt * ncols].rearrange("p (k n) -> p k n", k=kt)
    C.S.dma("pool", stream, lambda e: e.dma_start(out=dst, in_=view), writes=[r])
    return dst, r


def setup_consts(C, cst_bf_d, cst_f_d):
    nc, S, es = C.nc, C.S, C.es
    ncb = cst_bf_d.shape[1]
    ncf = cst_f_d.shape[1]
    C.cb = sbt(C, es, "cstb", [128, ncb], BF16)
    C.cf = sbt(C, es, "cstf", [128, ncf], F32)
    C.rcst = Res("cst")
    S.dma("pool", "dma:cstb", lambda e: e.dma_start(out=C.cb[:], in_=cst_bf_d), writes=[C.rcst])
    S.dma("sp", "dma:cstf", lambda e: e.dma_start(out=C.cf[:], in_=cst_f_d), writes=[C.rcst])


def bank(C, i):
    return C.P[i], C.rP[i]


def phase_load_x(C, x_d, xT, rx, tag):
    S = C.S
    with ExitStack() as es:
        stage = sbt(C, es, "xstage", [128, 2, 1024], F32)
        rst = [Res(), Res()]
        identf = C.cf[:, C.CF_IDENT:C.CF_IDENT + 128]
        for Tt in range(NT):
            b = Tt % 2
            S.dma("sp", "dma:%s%d" % (tag, b),
                  lambda e, b=b, Tt=Tt: e.dma_start(out=stage[:, b, :], in_=x_d[Tt * 128:(Tt + 1) * 128, :]),
                  writes=[rst[b]])
            for half in range(2):
                bk = (Tt * 2 + half) % 4
                P, rP = bank(C, bk)
                for j in range(4):
                    dt = half * 4 + j
                    S.op("pe", lambda e, P=P, j=j, b=b, dt=dt: e.transpose(
                        out=P[:, j * 128:(j + 1) * 128], in_=stage[:, b, dt * 128:(dt + 1) * 128], identity=identf),
                        reads=[rst[b], C.rcst], writes=[rP])
                eng = "act" if half == 0 else "dve"
                outv = xT[:, half * 4:half * 4 + 4, Tt * 128:(Tt + 1) * 128]
                inv = P[:].rearrange("p (a b) -> p a b", a=4)
                wr = [rx[half * 4 + j][Tt] for j in range(4)]
                if eng == "act":
                    S.op("act", lambda e, outv=outv, inv=inv: e.copy(out=outv, in_=inv), reads=[rP], writes=wr)
                else:
                    S.op("dve", lambda e, outv=outv, inv=inv: e.tensor_copy(out=outv, in_=inv), reads=[rP], writes=wr)
        S.barrier_all()
        S.emit()


def phase_store_x(C, xT, rx, out_d):
    S = C.S
    with ExitStack() as es:
        stage = sbt(C, es, "ostage", [128, 2, 1024], F32)
        rst = [Res(), Res()]
        identf = C.cf[:, C.CF_IDENT:C.CF_IDENT + 128]
        for Tt in range(NT):
            b = Tt % 2
            for half in range(2):
                bk = (Tt * 2 + half) % 4
                P, rP = bank(C, bk)
                for j in range(4):
                    dt = half * 4 + j
                    S.op("pe", lambda e, P=P, j=j, dt=dt, Tt=Tt: e.transpose(
                        out=P[:, j * 128:(j + 1) * 128], in_=xT[:, dt, Tt * 128:(Tt + 1) * 128], identity=identf),
                        reads=[rx[dt][Tt], C.rcst], writes=[rP])
                outv = stage[:, b, half * 512:(half + 1) * 512]
                if half == 0:
                    S.op("act", lambda e, outv=outv, P=P: e.copy(out=outv, in_=P[:]), reads=[rP], writes=[rst[b]])
                else:
                    S.op("dve", lambda e, outv=outv, P=P: e.tensor_copy(out=outv, in_=P[:]), reads=[rP], writes=[rst[b]])
            S.dma("sp", "dma:out%d" % b,
                  lambda e, b=b, Tt=Tt: e.dma_start(out=out_d[Tt * 128:(Tt + 1) * 128, :], in_=stage[:, b, :]),
                  reads=[rst[b]])
        S.final_wait("sp", ["dma:out0", "dma:out1"])
        S.barrier_all()
        S.emit()


def alloc_adaln(C, nv):
    es = C.es
    C.modT = sbt(C, es, "modT", [128, 2, 48], F32)
    C.rmod = Res("mod")
    C.vec = sbt(C, es, "vec", [128, nv], F32)
    C.rvec = Res("vec")
    C.Gt = sbt(C, es, "Gt", [128, 2, 2, 8], F32)


def phase_adaln(C, cvec_d, ada_w_d, vecs_d):
    S, es, nc = C.S, C.es, C.nc
    if not hasattr(C, "modT"):
        alloc_adaln(C, vecs_d.shape[1])
    S.dma("sp", "dma:vec", lambda e: e.dma_start(out=C.vec[:], in_=vecs_d), writes=[C.rvec])
    with ExitStack() as ps:
        cv = sbt(C, ps, "cv", [128, 8], F32)
        cact = sbt(C, ps, "cact", [128, 8], BF16)
        rowb = sbt(C, ps, "rowb", [1, 2, 512], F32)
        rcv, rcact, rrow = Res(), Res(), [Res(), Res()]
        S.dma("sp", "dma:cv", lambda e: e.dma_start(out=cv[:], in_=cvec_d), writes=[rcv])
        S.op("act", lambda e: e.activation(out=cact[:], in_=cv[:], func=AF.Silu), reads=[rcv], writes=[rcact])
        one11 = C.cf[0:1, C.CF_ONE:C.CF_ONE + 1]
        for l in range(2):
            wv = ada_w_d[l].rearrange("(kt p) n -> p kt n", p=128)
            Pm, rPm = bank(C, 2 + l)
            for cbk in range(12):
                w, rw = load_w(C, wv[:, :, cbk * 512:(cbk + 1) * 512], 8, 512)
                P, rP = bank(C, cbk % 2)
                rb_ = cbk % 2
                for kt in range(8):
                    S.op("pe", lambda e, P=P, w=w, kt=kt: e.matmul(
                        P[0:1, :], lhsT=cact[:, kt:kt + 1], rhs=w[:, kt, :], start=(kt == 0), stop=(kt == 7)),
                        reads=[rw, rcact], writes=[rP])
                S.op("dve", lambda e, P=P, rb_=rb_: e.tensor_copy(out=rowb[0:1, rb_, :], in_=P[0:1, :]),
                     reads=[rP], writes=[rrow[rb_]])
                for j in range(4):
                    blk = cbk * 4 + j
                    S.op("pe", lambda e, blk=blk, j=j, rb_=rb_, Pm=Pm: e.matmul(
                        Pm[:, blk:blk + 1], lhsT=rowb[0:1, rb_, j * 128:(j + 1) * 128], rhs=one11, start=True, stop=True),
                        reads=[rrow[rb_], C.rcst], writes=[rPm])
            adab = C.vec[:, C.V_ADAB + l * 48:C.V_ADAB + (l + 1) * 48]
            S.op("dve", lambda e, l=l, Pm=Pm, adab=adab: e.tensor_tensor(out=C.modT[:, l, :], in0=Pm[:, 0:48], in1=adab, op=ALU.add),
                 reads=[rPm, C.rvec], writes=[C.rmod])
            for n in range(2):
                sc = C.modT[:, l, (3 * n + 1) * 8:(3 * n + 1) * 8 + 8]
                ng = C.vec[:, C.V_NG + (l * 2 + n) * 8:C.V_NG + (l * 2 + n) * 8 + 8]
                S.op("dve", lambda e, l=l, n=n, sc=sc, ng=ng: e.scalar_tensor_tensor(
                    out=C.Gt[:, l, n, :], in0=sc, scalar=1.0, in1=ng, op0=ALU.add, op1=ALU.mult),
                    reads=[C.rmod, C.rvec], writes=[C.rmod])
        S.barrier_all()
        S.emit()


def mod_col(C, l, j):
    return C.modT[:, l, j * 8:(j + 1) * 8]


def phase_norm(C, xT, rx, G, SH, hT, rh, nblk=4, emit=True):
    S = C.S
    with ExitStack() as es:
        sq = sbt(C, es, "sq", [128, 2, 8, 512], BF16)
        rstd = sbt(C, es, "rstd", [128, 2, 512], F32)
        tmp = sbt(C, es, "ntmp", [128, 2, 512], F32)
        rsq, rrs, rtmp = [Res(), Res()], [Res(), Res()], [Res(), Res()]
        onesm = C.cb[:, C.CB_ONESM:C.CB_ONESM + 128]
        for tb in range(nblk):
            b = tb % 2
            ts = slice(tb * 512, (tb + 1) * 512)
            rxs = [rx[dt][tb * 4 + k] for dt in range(8) for k in range(4)]
            S.op("act", lambda e, b=b, ts=ts: e.activation(out=sq[:, b, :, :], in_=xT[:, :, ts], func=AF.Square),
                 reads=rxs, writes=[rsq[b]])
            P, rP = bank(C, 6 + b)
            for dt in range(8):
                S.op("pe", lambda e, P=P, b=b, dt=dt: e.matmul(P[:], lhsT=onesm, rhs=sq[:, b, dt, :], start=(dt == 0), stop=(dt == 7)),
                     reads=[rsq[b], C.rcst], writes=[rP])
            S.op("act", lambda e, P=P, b=b: e.activation(out=rstd[:, b, :], in_=P[:], func=AF.Ln, bias=C.cf[:, C.CF_EPS:C.CF_EPS + 1]),
                 reads=[rP, C.rcst], writes=[rrs[b]])
            S.op("act", lambda e, b=b: e.activation(out=rstd[:, b, :], in_=rstd[:, b, :], func=AF.Exp, scale=-0.5),
                 reads=[rrs[b]], writes=[rrs[b]])
            for dt in range(8):
                bb = dt % 2
                S.op("dve", lambda e, bb=bb, dt=dt, ts=ts, b=b: e.tensor_tensor(
                    out=tmp[:, bb, :], in0=xT[:, dt, ts], in1=rstd[:, b, :], op=ALU.mult),
                    reads=[rrs[b]] + [rx[dt][tb * 4 + k] for k in range(4)], writes=[rtmp[bb]])
                S.op("act", lambda e, bb=bb, dt=dt, ts=ts: e.activation(
                    out=hT[:, dt, ts], in_=tmp[:, bb, :], func=AF.Identity, scale=G[:, dt:dt + 1], bias=SH[:, dt:dt + 1]),
                    reads=[rtmp[bb], C.rmod], writes=[rh[dt][tb]])
        S.barrier_all()
        if emit:
            S.emit()


BIG = 1.0e4


def phase_router(C, hT, rh, comb, rcomb):
    S = C.S
    with ExitStack() as es:
        rwb = C.cb[:, C.CB_RW:C.CB_RW + 128].rearrange("p (k e) -> p k e", k=8)
        Pr, rPr = bank(C, 7)
        for Tt in range(NT):
            for dt in range(8):
                S.op("pe", lambda e, Tt=Tt, dt=dt: e.matmul(
                    Pr[:, Tt * 16:(Tt + 1) * 16], lhsT=hT[:, dt, Tt * 128:(Tt + 1) * 128], rhs=rwb[:, dt, :],
                    start=(dt == 0), stop=(dt == 7)),
                    reads=[rh[dt][Tt // 4], C.rcst], writes=[rPr])
        n = [0]

        def tmp(shape):
            n[0] += 1
            return sbt(C, es, "rt%d" % n[0], shape, F32)

        rr = Res("router_tmp")
        sc = tmp([128, 256])
        sel = tmp([128, 256])
        S.op("act", lambda e: e.activation(out=sc[:], in_=Pr[:, 0:256], func=AF.Sigmoid), reads=[rPr], writes=[rr])
        rb = C.vec[:, C.V_RB:C.V_RB + 16]
        sc3 = sc[:].rearrange("p (t e) -> p t e", e=16)
        sel3 = sel[:].rearrange("p (t e) -> p t e", e=16)
        sel4 = sel[:].rearrange("p (a k) -> p a k", k=4)

        def dv(fn):
            S.op("dve", fn, reads=[rr, C.rvec], writes=[rr])

        dv(lambda e: e.tensor_tensor(out=sel3, in0=sc3, in1=rb.unsqueeze(1).to_broadcast([128, 16, 16]), op=ALU.add))
        m1 = tmp([128, 64]); m2 = tmp([128, 64]); eq1 = tmp([128, 256]); sel2 = tmp([128, 256])
        eq14 = eq1[:].rearrange("p (a k) -> p a k", k=4)
        sel24 = sel2[:].rearrange("p (a k) -> p a k", k=4)
        dv(lambda e: e.tensor_reduce(out=m1[:], in_=sel4, axis=AX.X, op=ALU.max))
        dv(lambda e: e.tensor_tensor(out=eq14, in0=sel4, in1=m1[:].unsqueeze(2).to_broadcast([128, 64, 4]), op=ALU.is_equal))
        dv(lambda e: e.scalar_tensor_tensor(out=sel2[:], in0=eq1[:], scalar=-BIG, in1=sel[:], op0=ALU.mult, op1=ALU.add))
        dv(lambda e: e.tensor_reduce(out=m2[:], in_=sel24, axis=AX.X, op=ALU.max))
        gs = tmp([128, 64])
        dv(lambda e: e.tensor_tensor(out=gs[:], in0=m1[:], in1=m2[:], op=ALU.add))
        gs3 = gs[:].rearrange("p (t g) -> p t g", g=4)
        gmax = tmp([128, 16]); pen = tmp([128, 64])
        pen3 = pen[:].rearrange("p (t g) -> p t g", g=4)
        dv(lambda e: e.tensor_reduce(out=gmax[:], in_=gs3, axis=AX.X, op=ALU.max))
        dv(lambda e: e.tensor_tensor(out=pen3, in0=gs3, in1=gmax[:].unsqueeze(2).to_broadcast([128, 16, 4]), op=ALU.is_equal))
        dv(lambda e: e.tensor_scalar(out=pen[:], in0=pen[:], scalar1=BIG, scalar2=-BIG, op0=ALU.mult, op1=ALU.add))
        selg = tmp([128, 256])
        selg4 = selg[:].rearrange("p (a k) -> p a k", k=4)
        selg3 = selg[:].rearrange("p (t e) -> p t e", e=16)
        dv(lambda e: e.tensor_tensor(out=selg4, in0=sel4, in1=pen[:].unsqueeze(2).to_broadcast([128, 64, 4]), op=ALU.add))
        t1 = tmp([128, 16]); e1 = tmp([128, 256]); selg2 = tmp([128, 256]); t2 = tmp([128, 16]); e2 = tmp([128, 256])
        e13 = e1[:].rearrange("p (t e) -> p t e", e=16)
        e23 = e2[:].rearrange("p (t e) -> p t e", e=16)
        selg23 = selg2[:].rearrange("p (t e) -> p t e", e=16)
        dv(lambda e: e.tensor_reduce(out=t1[:], in_=selg3, axis=AX.X, op=ALU.max))
        dv(lambda e: e.tensor_tensor(out=e13, in0=selg3, in1=t1[:].unsqueeze(2).to_broadcast([128, 16, 16]), op=ALU.is_equal))
        dv(lambda e: e.scalar_tensor_tensor(out=selg2[:], in0=e1[:], scalar=-BIG, in1=selg[:], op0=ALU.mult, op1=ALU.add))
        dv(lambda e: e.tensor_reduce(out=t2[:], in_=selg23, axis=AX.X, op=ALU.max))
        dv(lambda e: e.tensor_tensor(out=e23, in0=selg23, in1=t2[:].unsqueeze(2).to_broadcast([128, 16, 16]), op=ALU.is_equal))
        dv(lambda e: e.tensor_tensor(out=e1[:], in0=e1[:], in1=e2[:], op=ALU.add))
        dv(lambda e: e.tensor_tensor(out=e1[:], in0=e1[:], in1=sc[:], op=ALU.mult))
        den = tmp([128, 16])
        dv(lambda e: e.tensor_reduce(out=den[:], in_=e13, axis=AX.X, op=ALU.add))
        dv(lambda e: e.reciprocal(out=den[:], in_=den[:]))
        S.op("dve", lambda e: e.tensor_tensor(out=comb[:], in0=e13, in1=den[:].unsqueeze(2).to_broadcast([128, 16, 16]), op=ALU.mult),
             reads=[rr], writes=[rcomb])
        S.barrier_all()
        S.emit()


def phase_moe(C, l, wg_d, wu_d, wd_d, hT, rh, xT, rx, comb, rcomb, g2):
    S = C.S
    with ExitStack() as es:
        cbt = sbt(C, es, "cbt", [128, 2, T], BF16)
        rcb = [Res(), Res()]
        sgt = sbt(C, es, "sgt", [128, 2, 512], BF16)
        rsg = [Res(), Res()]
        tt = sbt(C, es, "tt", [128, 2, 512], BF16)
        rtt = [Res(), Res()]
        hw = sbt(C, es, "hw", [128, 2, 4, 512], BF16)
        rhw = [[Res() for _ in range(4)] for _ in range(2)]
        identb = C.cb[:, C.CB_IDENT:C.CB_IDENT + 128]
        cnt_gu = 0
        cnt_d = 0
        cnt_hw = 0
        pending = [None]
        for ex in range(16):
            cb_i = ex % 2
            Pc, rPc = bank(C, 6)
            for q4 in range(4):
                for j in range(4):
                    Tt = q4 * 4 + j
                    S.op("pe", lambda e, Tt=Tt, j=j, ex=ex: e.matmul(
                        Pc[:, j * 128:(j + 1) * 128], lhsT=comb[:, Tt, ex:ex + 1].to_broadcast([128, 128]), rhs=identb,
                        start=True, stop=True), reads=[rcomb, C.rcst], writes=[rPc])
                S.op("act", lambda e, q4=q4, cb_i=cb_i: e.copy(out=cbt[:, cb_i, q4 * 512:(q4 + 1) * 512], in_=Pc[:]),
                     reads=[rPc], writes=[rcb[cb_i]])
            wg, rwg = load_w(C, wg_d[ex].rearrange("(kt p) n -> p kt n", p=128), 8, 512)
            wu, rwu = load_w(C, wu_d[ex].rearrange("(kt p) n -> p kt n", p=128), 8, 512)
            wd, rwd = load_w(C, wd_d[ex].rearrange("(kt p) n -> p kt n", p=128), 4, 1024)
            for tb in range(4):
                ts = slice(tb * 512, (tb + 1) * 512)
                hb = cnt_hw % 2
                cnt_hw += 1
                for f in range(4):
                    gb = cnt_gu % 2
                    cnt_gu += 1
                    Pg, rPg = bank(C, gb * 2)
                    Pu, rPu = bank(C, gb * 2 + 1)
                    for kt in range(8):
                        S.op("pe", lambda e, Pg=Pg, wg=wg, kt=kt, f=f, ts=ts: e.matmul(
                            Pg[:], lhsT=wg[:, kt, f * 128:(f + 1) * 128], rhs=hT[:, kt, ts], start=(kt == 0), stop=(kt == 7)),
                            reads=[rwg, rh[kt][tb]], writes=[rPg])
                    for kt in range(8):
                        S.op("pe", lambda e, Pu=Pu, wu=wu, kt=kt, f=f, ts=ts: e.matmul(
                            Pu[:], lhsT=wu[:, kt, f * 128:(f + 1) * 128], rhs=hT[:, kt, ts], start=(kt == 0), stop=(kt == 7)),
                            reads=[rwu, rh[kt][tb]], writes=[rPu])
                    S.op("act", lambda e, Pg=Pg, gb=gb: e.activation(out=sgt[:, gb, :], in_=Pg[:], func=AF.Silu),
                         reads=[rPg], writes=[rsg[gb]])
                    S.op("dve", lambda e, Pu=Pu, gb=gb: e.tensor_tensor(out=tt[:, gb, :], in0=Pu[:], in1=sgt[:, gb, :], op=ALU.mult),
                         reads=[rPu, rsg[gb]], writes=[rtt[gb]])
                    S.op("dve", lambda e, gb=gb, hb=hb, f=f, cb_i=cb_i, ts=ts: e.tensor_tensor(
                        out=hw[:, hb, f, :], in0=tt[:, gb, :], in1=cbt[:, cb_i, ts], op=ALU.mult),
                        reads=[rtt[gb], rcb[cb_i]], writes=[rhw[hb][f]])
                def down_unit(wd=wd, rwd=rwd, hb=hb, tb=tb, ts=ts):
                    nonlocal cnt_d
                    for dt in range(8):
                        db = cnt_d % 2
                        cnt_d += 1
                        Pd, rPd = bank(C, 4 + db)
                        for f in range(4):
                            S.op("pe", lambda e, Pd=Pd, wd=wd, f=f, dt=dt, hb=hb: e.matmul(
                                Pd[:], lhsT=wd[:, f, dt * 128:(dt + 1) * 128], rhs=hw[:, hb, f, :], start=(f == 0), stop=(f == 3)),
                                reads=[rwd, rhw[hb][f]], writes=[rPd])
                        rxs = [rx[dt][tb * 4 + k] for k in range(4)]
                        S.op("dve", lambda e, Pd=Pd, dt=dt, ts=ts: e.scalar_tensor_tensor(
                            out=xT[:, dt, ts], in0=Pd[:], scalar=g2[:, dt:dt + 1], in1=xT[:, dt, ts], op0=ALU.mult, op1=ALU.add),
                            reads=[rPd, C.rmod] + rxs, writes=rxs)

                if pending[0] is not None:
                    pending[0]()
                pending[0] = down_unit
        if pending[0] is not None:
            pending[0]()
        S.barrier_all()
        S.emit()


SEQ = 4096
NTF = SEQ // 128


def load_hblk(C, hfull_d, tb, hblk, rhblk, cnt):
    b = cnt[0] % 2
    cnt[0] += 1
    h_all, rhall = hfull_d
    r_, tl = tb // 4, tb % 4
    r0 = (((tl // 2) * 2 + r_) * 2 + tl % 2) * 128
    C.S.dma("sp", "dma:hblk%d" % b, lambda e, b=b, r0=r0: e.dma_start(out=hblk[:, b, :], in_=h_all[r0:r0 + 128, :]),
            reads=[rhall], writes=[rhblk[b]])
    return hblk[:, b, :].rearrange("p (k n) -> p k n", k=8), rhblk[b]


def rstd_from_ps(C, P, rP, out, rout):
    S = C.S
    S.op("act", lambda e: e.activation(out=out, in_=P, func=AF.Ln, bias=C.cf[:, C.CF_EPS:C.CF_EPS + 1]),
         reads=[rP, C.rcst], writes=[rout])
    S.op("act", lambda e: e.activation(out=out, in_=out, func=AF.Exp, scale=-0.5), reads=[rout], writes=[rout])


def proj_fm(C, w, rw, c0, M, hb, rhb, P, rP):
    for kt in range(8):
        C.S.op("pe", lambda e, kt=kt: e.matmul(P[0:M, :], lhsT=w[:, kt, c0:c0 + M], rhs=hb[:, kt, :], start=(kt == 0), stop=(kt == 7)),
               reads=[rw, rhb], writes=[rP])


def proj_tok(C, w, rw, c0, N, hb, rhb, j, P, rP):
    for kt in range(8):
        C.S.op("pe", lambda e, kt=kt: e.matmul(P[:, 0:N], lhsT=hb[:, kt, j * 128:(j + 1) * 128], rhs=w[:, kt, c0:c0 + N], start=(kt == 0), stop=(kt == 7)),
               reads=[rw, rhb], writes=[rP])


def phase_ret(C, hfull_d, wret_d, tab_d, out_d):
    S = C.S
    with ExitStack() as es:
        qT = sbt(C, es, "qT", [128, 2, SEQ], BF16)
        kT = sbt(C, es, "kT", [128, 2, SEQ], BF16)
        sgT = sbt(C, es, "sgT", [128, 2, SEQ], BF16)
        v_tok = sbt(C, es, "vtok", [128, NTF, 2, 128], BF16)
        rq_ = [[Res() for _ in range(8)] for _ in range(2)]
        rk_ = [[Res() for _ in range(8)] for _ in range(2)]
        rsg_ = [[Res() for _ in range(8)] for _ in range(2)]
        rvt = [[Res() for _ in range(NTF)] for _ in range(2)]
        with ExitStack() as e1:
            hblk = sbt(C, e1, "hblk", [128, 2, 4096], BF16)
            rhblk = [Res(), Res()]
            hcnt = [0]
            cs = sbt(C, e1, "cs", [128, 2, 2, 512], F32)
            rcs = [Res(), Res()]
            t1 = sbt(C, e1, "rt1", [128, 2, 512], F32)
            t2 = sbt(C, e1, "rt2", [128, 2, 512], F32)
            rt1, rt2 = [Res(), Res()], [Res(), Res()]
            ccnt = 0
            for h2 in range(2):
                wv = wret_d[h2].rearrange("(kt p) n -> p kt n", p=128)
                wA, rwA = load_w(C, wv[:, :, 0:384], 8, 384)
                wB, rwB = load_w(C, wv[:, :, 384:768], 8, 384)

                def wcol(g, wA=wA, rwA=rwA, wB=wB, rwB=rwB):
                    if g < 3:
                        return wA, rwA, g * 128
                    return wB, rwB, (g - 3) * 128

                for tb in range(8):
                    hb, rhb = load_hblk(C, hfull_d, tb, hblk, rhblk, hcnt)
                    cb_ = ccnt % 2
                    ccnt += 1
                    ts = slice(tb * 512, (tb + 1) * 512)
                    for ci, nm in enumerate(("cosT", "sinT")):
                        src = tab_d[nm][:, ts]
                        S.dma("sp", "dma:cs%d" % cb_, lambda e, cb_=cb_, ci=ci, src=src: e.dma_start(out=cs[:, cb_, ci, :], in_=src), writes=[rcs[cb_]])
                    for which in range(2):
                        Pa, rPa = bank(C, 0 + 2 * which)
                        Pb, rPb = bank(C, 1 + 2 * which)
                        w, rw, c0 = wcol(2 * which)
                        proj_fm(C, w, rw, c0, 128, hb, rhb, Pa, rPa)
                        w, rw, c0 = wcol(2 * which + 1)
                        proj_fm(C, w, rw, c0, 128, hb, rhb, Pb, rPb)
                        bb = which
                        S.op("dve", lambda e, Pa=Pa, bb=bb, cb_=cb_: e.tensor_tensor(out=t1[:, bb, :], in0=Pa[:], in1=cs[:, cb_, 0, :], op=ALU.mult),
                             reads=[rPa, rcs[cb_]], writes=[rt1[bb]])
                        S.op("dve", lambda e, Pb=Pb, bb=bb, cb_=cb_: e.tensor_tensor(out=t2[:, bb, :], in0=Pb[:], in1=cs[:, cb_, 1, :], op=ALU.mult),
                             reads=[rPb, rcs[cb_]], writes=[rt2[bb]])
                        dst = qT if which == 0 else kT
                        rdst = rq_ if which == 0 else rk_
                        S.op("dve", lambda e, bb=bb, h2=h2, ts=ts, dst=dst: e.tensor_tensor(out=dst[:, h2, ts], in0=t1[:, bb, :], in1=t2[:, bb, :], op=ALU.add),
                             reads=[rt1[bb], rt2[bb]], writes=[rdst[h2][tb]])
                    Pg, rPg = bank(C, 4)
                    w, rw, c0 = wcol(4)
                    proj_fm(C, w, rw, c0, 128, hb, rhb, Pg, rPg)
                    S.op("act", lambda e, h2=h2, ts=ts, Pg=Pg: e.activation(out=sgT[:, h2, ts], in_=Pg[:], func=AF.Silu), reads=[rPg], writes=[rsg_[h2][tb]])
                    for j in range(4):
                        Tt = tb * 4 + j
                        Pt, rPt = bank(C, 5 + (j % 2))
                        w, rw, c0 = wcol(5)
                        proj_tok(C, w, rw, c0, 128, hb, rhb, j, Pt, rPt)
                        S.op("act", lambda e, Tt=Tt, h2=h2, Pt=Pt: e.copy(out=v_tok[:, Tt, h2, :], in_=Pt[:, 0:128]), reads=[rPt], writes=[rvt[h2][Tt]])
            S.barrier_all()
            S.emit()
        with ExitStack() as e2:
            base4 = sbt(C, e2, "base4", [128, 2, 512], F32)
            diag = sbt(C, e2, "diag", [128, 2, 4, 512], F32)
            rtab = Res()
            S.dma("sp", "dma:base4", lambda e: e.dma_start(out=base4[:], in_=tab_d["base4"].rearrange("h p n -> p h n")), writes=[rtab])
            for h2 in range(2):
                S.dma("sp", "dma:diag", lambda e, h2=h2: e.dma_start(out=diag[:, h2, :, :], in_=tab_d["diag"][h2].rearrange("r p n -> p r n")), writes=[rtab])
            AT = sbt(C, e2, "AT", [128, 2, 512], BF16)
            rAT = [Res(), Res()]
            sq = sbt(C, e2, "rsq", [128, 2, 512], BF16)
            rsq = [Res(), Res()]
            rs_ = sbt(C, e2, "rrs", [128, 2, 512], F32)
            rrs = [Res(), Res()]
            ost = sbt(C, e2, "ost", [128, 2, 512], F32)
            rost = [Res(), Res()]
            ones128 = C.cb[:, C.CB_ONES128:C.CB_ONES128 + 128]
            gpow = C.cf[:, C.CF_GPOW:C.CF_GPOW + 64].rearrange("p (h r) -> p h r", h=2)
            acnt = 0
            ncnt = 0
            for qb in range(8):
                ts = slice(qb * 512, (qb + 1) * 512)
                for h2 in range(2):
                    Po, rPo = bank(C, ncnt % 2)
                    nk = 4 * qb + 4

                    def r_qk(kt):
                        nonlocal acnt
                        Pz, rPz = bank(C, 2 + (acnt % 2))
                        ab = acnt % 2
                        acnt += 1
                        S.op("pe", lambda e, Pz=Pz, h2=h2, kt=kt, ts=ts: e.matmul(Pz[:], lhsT=kT[:, h2, kt * 128:(kt + 1) * 128], rhs=qT[:, h2, ts], start=True, stop=True),
                             reads=[rk_[h2][kt // 4], rq_[h2][qb]], writes=[rPz])
                        return Pz, rPz, ab

                    nxt = r_qk(0)
                    for kt in range(nk):
                        r = kt - 4 * qb
                        Pz, rPz, ab = nxt
                        if r < 0:
                            S.op("dve", lambda e, Pz=Pz, ab=ab, h2=h2, r=r: e.scalar_tensor_tensor(
                                out=AT[:, ab, :], in0=Pz[:], scalar=gpow[:, h2, -r:-r + 1], in1=base4[:, h2, :], op0=ALU.mult, op1=ALU.mult),
                                reads=[rPz, rtab, C.rcst], writes=[rAT[ab]])
                        else:
                            S.op("dve", lambda e, Pz=Pz, ab=ab, h2=h2, r=r: e.tensor_tensor(out=AT[:, ab, :], in0=Pz[:], in1=diag[:, h2, r, :], op=ALU.mult),
                                 reads=[rPz, rtab], writes=[rAT[ab]])
                        if kt + 1 < nk:
                            nxt = r_qk(kt + 1)
                        S.op("pe", lambda e, Po=Po, kt=kt, h2=h2, ab=ab, nk=nk: e.matmul(Po[:], lhsT=v_tok[:, kt, h2, :], rhs=AT[:, ab, :], start=(kt == 0), stop=(kt == nk - 1)),
                             reads=[rvt[h2][kt], rAT[ab]], writes=[rPo])
                    b = ncnt % 2
                    ncnt += 1
                    S.op("act", lambda e, Po=Po, b=b: e.activation(out=sq[:, b, :], in_=Po[:], func=AF.Square), reads=[rPo], writes=[rsq[b]])
                    Pm, rPm = bank(C, 6 + b)
                    S.op("pe", lambda e, Pm=Pm, b=b: e.matmul(Pm[:], lhsT=ones128, rhs=sq[:, b, :], start=True, stop=True), reads=[rsq[b], C.rcst], writes=[rPm])
                    rstd_from_ps(C, Pm[:], rPm, rs_[:, b, :], rrs[b])
                    S.op("dve", lambda e, Po=Po, b=b: e.tensor_tensor(out=rs_[:, b, :], in0=Po[:], in1=rs_[:, b, :], op=ALU.mult), reads=[rPo, rrs[b]], writes=[rrs[b]])
                    S.op("dve", lambda e, b=b, h2=h2, ts=ts: e.tensor_tensor(out=ost[:, b, :], in0=rs_[:, b, :], in1=sgT[:, h2, ts], op=ALU.mult),
                         reads=[rrs[b], rsg_[h2][qb]], writes=[rost[b]])
                    o_my, romy = out_d
                    r0 = ((qb // 4) * 4 + h2) * 128
                    c0_ = (qb % 4) * 512
                    S.dma("pool", "dma:retout%d" % b, lambda e, b=b, r0=r0, c0_=c0_: e.dma_start(out=o_my[r0:r0 + 128, c0_:c0_ + 512], in_=ost[:, b, :]),
                          reads=[rost[b]], writes=[romy])
            S.barrier_all()
            S.emit()


def phase_att(C, hfull_d, watt_d, bias5_d, out_d, half):
    S = C.S
    with ExitStack() as es:
        aqT = sbt(C, es, "aqT", [64, 2, SEQ], BF16)
        akT = sbt(C, es, "akT", [64, 2, SEQ], BF16)
        av_tok = sbt(C, es, "avtok", [128, NTF, 128], BF16)
        raq = [[Res() for _ in range(8)] for _ in range(2)]
        rak = [[Res() for _ in range(8)] for _ in range(2)]
        rav = [Res() for _ in range(NTF)]
        ones64 = C.cb[0:64, C.CB_ONES64:C.CB_ONES64 + 64]
        with ExitStack() as e1:
            hblk = sbt(C, e1, "hblk", [128, 2, 4096], BF16)
            rhblk = [Res(), Res()]
            hcnt = [0]
            sq = sbt(C, e1, "asq", [64, 2, 512], BF16)
            rsq = [Res(), Res()]
            rs_ = sbt(C, e1, "ars", [64, 2, 512], F32)
            rrs = [Res(), Res()]
            wv = watt_d.rearrange("(kt p) n -> p kt n", p=128)
            wC, rwC = load_w(C, wv[:, :, 0:256], 8, 256)
            wD, rwD = load_w(C, wv[:, :, 256:384], 8, 128)
            cnt = 0
            for tb in range(8):
                hb, rhb = load_hblk(C, hfull_d, tb, hblk, rhblk, hcnt)
                ts = slice(tb * 512, (tb + 1) * 512)
                for which in range(2):
                    for a in range(2):
                        b = cnt % 2
                        cnt += 1
                        Pq, rPq = bank(C, b)
                        proj_fm(C, wC, rwC, which * 128 + a * 64, 64, hb, rhb, Pq, rPq)
                        S.op("act", lambda e, Pq=Pq, b=b: e.activation(out=sq[:, b, :], in_=Pq[0:64, :], func=AF.Square), reads=[rPq], writes=[rsq[b]])
                        Pm, rPm = bank(C, 2 + b)
                        S.op("pe", lambda e, Pm=Pm, b=b: e.matmul(Pm[0:64, :], lhsT=ones64, rhs=sq[:, b, :], start=True, stop=True),
                             reads=[rsq[b], C.rcst], writes=[rPm])
                        S.op("act", lambda e, Pm=Pm, b=b: e.activation(out=rs_[:, b, :], in_=Pm[0:64, :], func=AF.Ln, bias=C.cf[0:64, C.CF_EPS:C.CF_EPS + 1]),
                             reads=[rPm, C.rcst], writes=[rrs[b]])
                        S.op("act", lambda e, b=b: e.activation(out=rs_[:, b, :], in_=rs_[:, b, :], func=AF.Exp, scale=-0.5), reads=[rrs[b]], writes=[rrs[b]])
                        dst = aqT if which == 0 else akT
                        rd = raq if which == 0 else rak
                        gcol = C.cf[0:64, C.CF_QG + which:C.CF_QG + which + 1]
                        S.op("dve", lambda e, Pq=Pq, b=b, dst=dst, a=a, ts=ts, gcol=gcol: e.scalar_tensor_tensor(
                            out=dst[:, a, ts], in0=Pq[0:64, :], scalar=gcol, in1=rs_[:, b, :], op0=ALU.mult, op1=ALU.mult),
                            reads=[rPq, rrs[b], C.rcst], writes=[rd[a][tb]])
                for j in range(4):
                    Tt = tb * 4 + j
                    Pt, rPt = bank(C, 4 + (j % 2))
                    proj_tok(C, wD, rwD, 0, 128, hb, rhb, j, Pt, rPt)
                    S.op("act", lambda e, Tt=Tt, Pt=Pt: e.copy(out=av_tok[:, Tt, :], in_=Pt[:, 0:128]), reads=[rPt], writes=[rav[Tt]])
            S.barrier_all()
            S.emit()
        with ExitStack() as e2:
            bias5 = sbt(C, e2, "bias5", [128, 2, 640], F32)
            rb5 = Res()
            S.dma("sp", "dma:bias5", lambda e: e.dma_start(out=bias5[:], in_=bias5_d), writes=[rb5])
            tmp = sbt(C, e2, "atmp", [128, 2, 640], F32)
            rtmp = [Res(), Res()]
            PT = sbt(C, e2, "aPT", [128, 2, 640], BF16)
            rPT = [Res(), Res()]
            rden = sbt(C, e2, "rden", [64, 2, 512], F32)
            rrd = [Res(), Res()]
            ost = sbt(C, e2, "aost", [64, 2, 512], BF16)
            rost = [Res(), Res()]
            one64 = C.cb[:, C.CB_ONE64:C.CB_ONE64 + 64]
            units = [(qb, a, jq) for qb in range(8) for a in range(2) for jq in range(4)]
            uinfo = {}

            def a_qk(ui):
                qb, a, jq = units[ui]
                qt = qb * 4 + jq
                b = ui % 2
                ZA, rZA = bank(C, b)
                ZB, rZB = bank(C, 2 + b)
                kmin = max(0, 4 - qt)
                qs = slice(qt * 128, (qt + 1) * 128)
                for kr in range(kmin, 5):
                    kt = qt - 4 + kr
                    Z, rZ, zo = (ZA, rZA, kr * 128) if kr < 4 else (ZB, rZB, 0)
                    S.op("pe", lambda e, Z=Z, zo=zo, a=a, kt=kt, qs=qs: e.matmul(
                        Z[:, zo:zo + 128], lhsT=akT[:, a, kt * 128:(kt + 1) * 128], rhs=aqT[:, a, qs], start=True, stop=True),
                        reads=[rak[a][kt // 4], raq[a][qb]], writes=[rZ])
                uinfo[ui] = (ZA, rZA, ZB, rZB, kmin, b, qt)

            def a_ew(ui):
                qb, a, jq = units[ui]
                ZA, rZA, ZB, rZB, kmin, b, qt = uinfo[ui]
                if kmin < 4:
                    S.op("dve", lambda e, ZA=ZA, b=b, a=a, kmin=kmin: e.scalar_tensor_tensor(
                        out=tmp[:, b, kmin * 128:512], in0=ZA[:, kmin * 128:512], scalar=0.125, in1=bias5[:, a, kmin * 128:512],
                        op0=ALU.mult, op1=ALU.add), reads=[rZA, rb5], writes=[rtmp[b]])
                S.op("dve", lambda e, ZB=ZB, b=b, a=a: e.scalar_tensor_tensor(
                    out=tmp[:, b, 512:640], in0=ZB[:, 0:128], scalar=0.125, in1=bias5[:, a, 512:640], op0=ALU.mult, op1=ALU.add),
                    reads=[rZB, rb5], writes=[rtmp[b]])
                S.op("act", lambda e, b=b, kmin=kmin: e.activation(out=PT[:, b, kmin * 128:640], in_=tmp[:, b, kmin * 128:640], func=AF.Exp),
                     reads=[rtmp[b]], writes=[rPT[b]])

            def a_pv(ui):
                qb, a, jq = units[ui]
                ZA, rZA, ZB, rZB, kmin, b, qt = uinfo[ui]
                ob = (qb * 2 + a) % 2
                Po, rPo = bank(C, 4 + ob)
                Pd, rPd = bank(C, 6 + ob)
                for kr in range(kmin, 5):
                    kt = qt - 4 + kr
                    S.op("pe", lambda e, Po=Po, jq=jq, kt=kt, a=a, b=b, kr=kr, kmin=kmin: e.matmul(
                        Po[0:64, jq * 128:(jq + 1) * 128], lhsT=av_tok[:, kt, a * 64:(a + 1) * 64], rhs=PT[:, b, kr * 128:(kr + 1) * 128],
                        start=(kr == kmin), stop=(kr == 4)), reads=[rav[kt], rPT[b]], writes=[rPo])
                    S.op("pe", lambda e, Pd=Pd, jq=jq, b=b, kr=kr, kmin=kmin: e.matmul(
                        Pd[0:64, jq * 128:(jq + 1) * 128], lhsT=one64, rhs=PT[:, b, kr * 128:(kr + 1) * 128],
                        start=(kr == kmin), stop=(kr == 4)), reads=[C.rcst, rPT[b]], writes=[rPd])
                if jq == 3:
                    ts = slice(qb * 512, (qb + 1) * 512)
                    S.op("dve", lambda e, Pd=Pd, ob=ob: e.reciprocal(out=rden[:, ob, :], in_=Pd[0:64, :]), reads=[rPd], writes=[rrd[ob]])
                    S.op("dve", lambda e, Po=Po, ob=ob: e.tensor_tensor(out=ost[:, ob, :], in0=Po[0:64, :], in1=rden[:, ob, :], op=ALU.mult),
                         reads=[rPo, rrd[ob]], writes=[rost[ob]])
                    o_my, romy = out_d
                    r0 = ((qb // 4) * 4 + 2 + half) * 128 + a * 64
                    c0_ = (qb % 4) * 512
                    S.dma("sp", "dma:attout%d" % ob, lambda e, ob=ob, r0=r0, c0_=c0_: e.dma_start(out=o_my[r0:r0 + 64, c0_:c0_ + 512], in_=ost[:, ob, :]),
                          reads=[rost[ob]], writes=[romy])

            a_qk(0)
            for ui in range(len(units)):
                a_ew(ui)
                if ui + 1 < len(units):
                    a_qk(ui + 1)
                a_pv(ui)
            S.barrier_all()
            S.emit()


def phase_sb(C, hfull_d, wsb_d, m01_d, out_d):
    S = C.S
    trineg = C.cb[:, C.CB_TRIN:C.CB_TRIN + 128]
    omtneg = C.cb[:, C.CB_OMTN:C.CB_OMTN + 128]
    onecol = C.cf[:, C.CF_ONE:C.CF_ONE + 1]
    with ExitStack() as es:
        m01 = sbt(C, es, "m01", [128, 2, 1024], BF16)
        rm01 = Res()
        S.dma("pool", "dma:m01", lambda e: e.dma_start(out=m01[:], in_=m01_d.rearrange("r p n -> p r n")), writes=[rm01])
        for grp in range(4):
            with ExitStack() as eg:
                qT = sbt(C, eg, "sqT", [64, 2, SEQ], BF16)
                kT = sbt(C, eg, "skT", [64, 2, SEQ], BF16)
                v_tok = sbt(C, eg, "svtok", [128, NTF, 128], BF16)
                rq = [[Res() for _ in range(8)] for _ in range(2)]
                rk = [[Res() for _ in range(8)] for _ in range(2)]
                rv = [Res() for _ in range(NTF)]
                with ExitStack() as e1:
                    hblk = sbt(C, e1, "hblk", [128, 2, 4096], BF16)
                    rhblk = [Res(), Res()]
                    hcnt = [0]
                    wv = wsb_d[grp].rearrange("(kt p) n -> p kt n", p=128)
                    wC, rwC = load_w(C, wv[:, :, 0:256], 8, 256)
                    wD, rwD = load_w(C, wv[:, :, 256:384], 8, 128)
                    cnt = 0
                    for tb in range(8):
                        hb, rhb = load_hblk(C, hfull_d, tb, hblk, rhblk, hcnt)
                        ts = slice(tb * 512, (tb + 1) * 512)
                        for which in range(2):
                            for a in range(2):
                                b = cnt % 4
                                cnt += 1
                                Pq, rPq = bank(C, b)
                                proj_fm(C, wC, rwC, which * 128 + a * 64, 64, hb, rhb, Pq, rPq)
                                dst = qT if which == 0 else kT
                                rd = rq if which == 0 else rk
                                if cnt % 2:
                                    S.op("act", lambda e, Pq=Pq, dst=dst, a=a, ts=ts: e.copy(out=dst[:, a, ts], in_=Pq[0:64, :]), reads=[rPq], writes=[rd[a][tb]])
                                else:
                                    S.op("dve", lambda e, Pq=Pq, dst=dst, a=a, ts=ts: e.tensor_copy(out=dst[:, a, ts], in_=Pq[0:64, :]), reads=[rPq], writes=[rd[a][tb]])
                        for j in range(4):
                            Tt = tb * 4 + j
                            Pt, rPt = bank(C, 4 + (j % 2))
                            proj_tok(C, wD, rwD, 0, 128, hb, rhb, j, Pt, rPt)
                            S.op("act", lambda e, Tt=Tt, Pt=Pt: e.copy(out=v_tok[:, Tt, :], in_=Pt[:, 0:128]), reads=[rPt], writes=[rv[Tt]])
                    S.barrier_all()
                    S.emit()
                with ExitStack() as e2:
                    NB = 3
                    eb = sbt(C, e2, "seb", [128, NB, 1024], BF16)
                    spb = sbt(C, e2, "sspb", [128, 2, 1024], BF16)
                    ecb = sbt(C, e2, "secb", [128, 2, 1024], BF16)
                    ab = sbt(C, e2, "sab", [128, 2, 1024], BF16)
                    reb = [Res() for _ in range(NB)]
                    rspb = [Res(), Res()]
                    recb = [Res(), Res()]
                    rab = [Res(), Res()]
                    ost = sbt(C, e2, "sost", [64, 2, 512], BF16)
                    rost = [Res(), Res()]
                    onesneg = C.cb[:, C.CB_ONESN:C.CB_ONESN + 128]
                    XX = C.PP[2]
                    XA, rXA = bank(C, 4)
                    XB, rXB = bank(C, 5)
                    zc = 0
                    ec = 0
                    pc = 0
                    oc = 0
                    for a in range(2):
                        for qb in range(8):
                            ts = slice(qb * 512, (qb + 1) * 512)
                            nk = 4 * qb + 4
                            npair = nk // 2
                            ob = oc % 2
                            oc += 1
                            O, rO = bank(C, 6 + ob)

                            zinfo = {}

                            def qk(pi):
                                nonlocal zc
                                k1 = nk - 1 - 2 * pi
                                zb = zc % 2
                                zc += 1
                                ZZ = C.PP[zb]
                                rZ = [C.rP[2 * zb], C.rP[2 * zb + 1]]
                                for hh_, kt in enumerate((k1, k1 - 1)):
                                    S.op("pe", lambda e, ZZ=ZZ, hh_=hh_, kt=kt, a=a, ts=ts: e.matmul(
                                        ZZ[:, hh_ * 512:(hh_ + 1) * 512], lhsT=kT[:, a, kt * 128:(kt + 1) * 128], rhs=qT[:, a, ts], start=True, stop=True),
                                        reads=[rk[a][kt // 4], rq[a][qb]], writes=[rZ[hh_]])
                                zinfo[pi] = (ZZ, rZ)

                            def e_op(pi):
                                nonlocal ec
                                ZZ, rZ = zinfo[pi]
                                ei = ec % NB
                                ec += 1
                                S.op("act", lambda e, ZZ=ZZ, ei=ei: e.activation(out=eb[:, ei, :], in_=ZZ[:], func=AF.Exp, scale=0.125), reads=rZ, writes=[reb[ei]])
                                return ei

                            def av(pi, pb):
                                k1 = nk - 1 - 2 * pi
                                for hh_, kt in enumerate((k1, k1 - 1)):
                                    S.op("pe", lambda e, O=O, kt=kt, pb=pb, hh_=hh_, pi=pi, a=a, npair=npair: e.matmul(
                                        O[0:64, :], lhsT=v_tok[:, kt, a * 64:(a + 1) * 64], rhs=ab[:, pb, hh_ * 512:(hh_ + 1) * 512],
                                        start=(pi == 0 and hh_ == 0), stop=(pi == npair - 1 and hh_ == 1)),
                                        reads=[rv[kt], rab[pb]], writes=[rO])

                            qk(0)
                            eis = {0: e_op(0)}
                            if npair > 1:
                                qk(1)
                            pbs = {}
                            for pi in range(npair):
                                k1 = nk - 1 - 2 * pi
                                k0 = k1 - 1
                                ei = eis[pi]
                                pb = pc % 2
                                pc += 1
                                pbs[pi] = pb
                                if k0 >= 4 * qb:
                                    mp = 0 if (k1 - 4 * qb) == 3 else 1
                                    S.op("dve", lambda e, ei=ei, mp=mp: e.tensor_tensor(out=eb[:, ei, :], in0=eb[:, ei, :], in1=m01[:, mp, :], op=ALU.mult),
                                         reads=[reb[ei], rm01], writes=[reb[ei]])
                                S.op("act", lambda e, ei=ei, pb=pb: e.activation(out=spb[:, pb, :], in_=eb[:, ei, :], func=AF.Ln, bias=onecol),
                                     reads=[reb[ei], C.rcst], writes=[rspb[pb]])
                                if pi + 1 < npair:
                                    eis[pi + 1] = e_op(pi + 1)
                                first = (pi == 0)
                                S.op("pe", lambda e, pb=pb, first=first: e.matmul(XA[:], lhsT=trineg, rhs=spb[:, pb, 0:512], start=first, stop=True),
                                     reads=[rspb[pb], C.rcst], writes=[rXA])
                                S.op("pe", lambda e, pb=pb, first=first: e.matmul(XB[:], lhsT=onesneg, rhs=spb[:, pb, 0:512], start=first, stop=True),
                                     reads=[rspb[pb], C.rcst], writes=[rXB])
                                S.op("pe", lambda e, pb=pb: e.matmul(XB[:], lhsT=trineg, rhs=spb[:, pb, 512:1024], start=False, stop=True),
                                     reads=[rspb[pb], C.rcst], writes=[rXB])
                                if pi >= 1:
                                    av(pi - 1, pbs[pi - 1])
                                if pi + 2 < npair:
                                    qk(pi + 2)
                                S.op("act", lambda e, pb=pb: e.activation(out=ecb[:, pb, :], in_=XX[:], func=AF.Exp), reads=[rXA, rXB], writes=[recb[pb]])
                                if pi + 1 < npair:
                                    S.op("pe", lambda e, pb=pb: e.matmul(XA[:], lhsT=omtneg, rhs=spb[:, pb, 0:512], start=False, stop=True),
                                         reads=[rspb[pb], C.rcst], writes=[rXA])
                                    S.op("pe", lambda e, pb=pb: e.matmul(XA[:], lhsT=onesneg, rhs=spb[:, pb, 512:1024], start=False, stop=True),
                                         reads=[rspb[pb], C.rcst], writes=[rXA])
                                    S.op("pe", lambda e, pb=pb: e.matmul(XB[:], lhsT=omtneg, rhs=spb[:, pb, 512:1024], start=False, stop=True),
                                         reads=[rspb[pb], C.rcst], writes=[rXB])
                                S.op("dve", lambda e, ei=ei, pb=pb: e.tensor_tensor(out=ab[:, pb, :], in0=eb[:, ei, :], in1=ecb[:, pb, :], op=ALU.mult),
                                     reads=[reb[ei], recb[pb]], writes=[rab[pb]])
                            av(npair - 1, pbs[npair - 1])
                            S.op("dve", lambda e, O=O, ob=ob: e.tensor_copy(out=ost[:, ob, :], in_=O[0:64, :]), reads=[rO], writes=[rost[ob]])
                            o_my, romy = out_d
                            r0 = ((qb // 4) * 4 + grp) * 128 + a * 64
                            c0_ = (qb % 4) * 512
                            S.dma("sp", "dma:sbout%d" % ob, lambda e, ob=ob, r0=r0, c0_=c0_: e.dma_start(out=o_my[r0:r0 + 64, c0_:c0_ + 512], in_=ost[:, ob, :]),
                                  reads=[rost[ob]], writes=[romy])
                    S.barrier_all()
                    S.emit()


def phase_h_out(C, hT, rh, out_d):
    S = C.S
    h_my, rhmy = out_d
    for tb in range(4):
        ts = slice(tb * 512, (tb + 1) * 512)
        S.dma("sp", "dma:hout", lambda e, tb=tb, ts=ts: e.dma_start(
            out=h_my[tb * 128:(tb + 1) * 128, :].rearrange("p (k n) -> p k n", k=8), in_=hT[:, :, ts]),
            reads=[rh[dt][tb] for dt in range(8)], writes=[rhmy])
    S.barrier_all()
    S.emit()


def phase_load_mix(C, mix_d, hT, rh, l):
    S = C.S
    o_all, roall = mix_d
    with ExitStack() as es:
        st = sbt(C, es, "mixst", [128, 2, 2, T], BF16)
        rst = [[Res(), Res()], [Res(), Res()]]
        tm = sbt(C, es, "mixtm", [128, 2, 2, T], BF16)
        rtm = [[Res(), Res()], [Res(), Res()]]
        for kt in range(8):
            if l == 0:
                r, fb = (kt // 2, kt % 2) if kt < 4 else ((kt - 4) // 2, 2 + (kt - 4) % 2)
            else:
                r, fb = kt // 4, kt % 4
            bb = kt % 2
            for sh in range(2):
                r0 = ((sh * 2 + r) * 4 + fb) * 128
                S.dma("sp", "dma:mix%d%d" % (bb, sh), lambda e, bb=bb, sh=sh, r0=r0: e.dma_start(out=st[:, bb, sh, :], in_=o_all[r0:r0 + 128, :]),
                      reads=[roall], writes=[rst[bb][sh]])
                S.op("act", lambda e, bb=bb, sh=sh: e.activation(out=tm[:, bb, sh, :], in_=st[:, bb, sh, :], func=AF.Identity,
                                                                 scale=C.vec[:, C.V_SEL + sh:C.V_SEL + sh + 1]),
                     reads=[rst[bb][sh], C.rvec], writes=[rtm[bb][sh]])
            S.op("dve", lambda e, bb=bb, kt=kt: e.tensor_tensor(out=hT[:, kt, :], in0=tm[:, bb, 0, :], in1=tm[:, bb, 1, :], op=ALU.add),
                 reads=[rtm[bb][0], rtm[bb][1]], writes=[rh[kt][tb] for tb in range(4)])
        S.barrier_all()
        S.emit()


def all_gather(C, src, rsrc, dst, rdst):
    n = src.shape[0] // 2
    for u in range(2):
        C.S.cc("pool", "cc", lambda e, u=u: e.collective_compute(
            "AllGather", ALU.bypass, replica_groups=[[0, 4], [1, 5], [2, 6], [3, 7]],
            ins=[src[u * n:(u + 1) * n, :]], outs=[dst[u * 2 * n:(u + 1) * 2 * n, :]]), reads=[rsrc], writes=[rdst])
    C.S.final_wait("pool", ["cc"])


def phase_wout(C, wout_d, mixT, rmix, xT, rx, g1):
    S = C.S
    wv = wout_d.rearrange("(kt p) n -> p kt n", p=128)
    n = 0
    for half in range(2):
        w, rw = load_w(C, wv[:, :, half * 512:(half + 1) * 512], 8, 512)
        for tb in range(4):
            ts = slice(tb * 512, (tb + 1) * 512)
            for c4 in range(4):
                dt = half * 4 + c4
                P, rP = bank(C, n % 4)
                n += 1
                for kt in range(8):
                    S.op("pe", lambda e, P=P, w=w, kt=kt, c4=c4, ts=ts: e.matmul(
                        P[:], lhsT=w[:, kt, c4 * 128:(c4 + 1) * 128], rhs=mixT[:, kt, ts], start=(kt == 0), stop=(kt == 7)),
                        reads=[rw, rmix[kt][tb]], writes=[rP])
                rxs = [rx[dt][tb * 4 + k] for k in range(4)]
                S.op("dve", lambda e, P=P, dt=dt, ts=ts: e.scalar_tensor_tensor(
                    out=xT[:, dt, ts], in0=P[:], scalar=g1[:, dt:dt + 1], in1=xT[:, dt, ts], op0=ALU.mult, op1=ALU.add),
                    reads=[rP, C.rmod] + rxs, writes=rxs)


CF_IDENT, CF_ONE, CF_EPS, CF_QG, CF_GPOW, NCF = 0, 128, 129, 130, 132, 196
CB_IDENT, CB_ONESM, CB_RW, CB_ONES128, CB_ONES64, CB_ONE64, CB_TRIN, CB_OMTN, CB_ONESN, NCB = 0, 128, 256, 384, 512, 576, 640, 768, 896, 1024
V_NG, V_RB, V_ADAB, V_SEL, NV = 0, 32, 48, 144, 146


def set_layout(C):
    C.CF_IDENT, C.CF_ONE, C.CF_EPS, C.CF_QG, C.CF_GPOW = CF_IDENT, CF_ONE, CF_EPS, CF_QG, CF_GPOW
    C.CB_IDENT, C.CB_ONESM, C.CB_RW, C.CB_ONES128, C.CB_ONES64, C.CB_ONE64, C.CB_TRIN, C.CB_OMTN, C.CB_ONESN = (
        CB_IDENT, CB_ONESM, CB_RW, CB_ONES128, CB_ONES64, CB_ONE64, CB_TRIN, CB_OMTN, CB_ONESN)
    C.V_NG, C.V_RB, C.V_ADAB, C.V_SEL = V_NG, V_RB, V_ADAB, V_SEL


def dram_in(nc, name, shape):
    return nc.dram_tensor(name, list(shape), F32, kind="ExternalInput").ap()


def dram_out(nc, name, shape):
    return nc.dram_tensor(name, list(shape), F32, kind="ExternalOutput").ap()


def new_nc():
    return bass.Bass("TRN2", target_bir_lowering=False)


def tok_state(C, es):
    xT = sbt(C, es, "xT", [128, 8, T], F32)
    rx = [[Res() for _ in range(NT)] for _ in range(8)]
    hT = sbt(C, es, "hT", [128, 8, T], BF16)
    rh = [[Res() for _ in range(4)] for _ in range(8)]
    return xT, rx, hT, rh


NIDX = 24


def build_fused(stop=None):
    nc = new_nc()
    I32 = mybir.dt.int32
    x_d = dram_in(nc, "x_own", [T, D]); cvec_d = dram_in(nc, "cvec", [128, 8]); ada_w_d = dram_in(nc, "ada_w", [2, 1024, 6144])
    vecs_d = dram_in(nc, "vecs", [128, NV]); cstb_d = dram_in(nc, "cstb", [128, NCB]); cstf_d = dram_in(nc, "cstf", [128, NCF])
    wret_d = dram_in(nc, "wret", [2, 1024, 768]); watt_d = dram_in(nc, "watt", [2, 1024, 384]); bias5_d = dram_in(nc, "bias5", [2, 128, 2, 640])
    tab = {k: dram_in(nc, k, shp) for k, shp in (("cosT", [128, SEQ]), ("sinT", [128, SEQ]), ("base4", [2, 128, 512]), ("diag", [2, 4, 128, 512]))}
    m01_d = dram_in(nc, "m01", [2, 128, 1024]); wsb_d = dram_in(nc, "wsb", [4, 1024, 384])
    wout_d = [dram_in(nc, "wout%d" % l, [1024, 1024]) for l in range(2)]
    wg_d = [dram_in(nc, "wg%d" % l, [16, 1024, 512]) for l in range(2)]
    wu_d = [dram_in(nc, "wu%d" % l, [16, 1024, 512]) for l in range(2)]
    wd_d = [dram_in(nc, "wd%d" % l, [16, 512, 1024]) for l in range(2)]
    out_d = dram_out(nc, "x_out", [T, D])
    h_my = nc.dram_tensor("h_my", [512, 4096], BF16).ap()
    h_all = nc.dram_tensor("h_all", [2 * 512, 4096], BF16).ap()
    o_my = nc.dram_tensor("o_my", [1024, 2048], BF16).ap()
    o_all = nc.dram_tensor("o_all", [2 * 1024, 2048], BF16).ap()
    rhmy, rhall, romy, roall = Res("h_my"), Res("h_all"), Res("o_my"), Res("o_all")
    with ExitStack() as es:
        C = mk_ctx(nc, es); set_layout(C)
        setup_consts(C, cstb_d, cstf_d)
        xT = sbt(C, es, "xT", [128, 8, T], F32)
        rx = [[Res() for _ in range(NT)] for _ in range(8)]
        alloc_adaln(C, NV)

        def tok_bufs(ph):
            hT = sbt(C, ph, "hT", [128, 8, T], BF16)
            rh = [[Res() for _ in range(4)] for _ in range(8)]
            comb = sbt(C, ph, "comb", [128, 16, 16], BF16)
            ring2 = sbt(C, ph, "ring2", [128, 2, 4096], BF16)
            del C.slots[NSLOT:]
            for i in range(2):
                C.slots.append((ring2[:, i, :], Res("ringx%d" % i), "dma:ringx%d" % i))
            return hT, rh, comb, Res()

        with ExitStack() as ph:
            hT, rh, comb, rcomb = tok_bufs(ph)
            phase_load_x(C, x_d, xT, rx, "xs")
            phase_adaln(C, cvec_d, ada_w_d, vecs_d)
            phase_norm(C, xT, rx, C.Gt[:, 0, 0, :], mod_col(C, 0, 0), hT, rh)
            phase_h_out(C, hT, rh, (h_my, rhmy))
            del C.slots[NSLOT:]
        for l in range(2):
            if stop == 0:
                phase_store_x(C, xT, rx, out_d)
                return nc
            all_gather(C, h_my, rhmy, h_all, rhall)
            if stop == 1:
                phase_store_x(C, xT, rx, out_d)
                return nc
            if stop in (11, 12):
                with ExitStack() as tt:
                    hblk = sbt(C, tt, "hblk", [128, 2, 4096], BF16)
                    rhblk = [Res(), Res()]
                    if stop == 11:
                        import os
                        var = os.environ.get("MK_VAR", "a")
                        if var == "a":
                            for tb_ in (5, 5, 5):
                                hb, rhb = load_hblk(C, (h_all, rhall), tb_, hblk, rhblk, [0])
                        elif var == "b":
                            hb, rhb = load_hblk(C, (h_all, rhall), 5, hblk, rhblk, [1])
                        elif var.startswith("t"):
                            hb, rhb = load_hblk(C, (h_all, rhall), int(var[1:]), hblk, rhblk, [0])
                    else:
                        C.S.dma("pool", "dma:hblk0", lambda e: e.indirect_dma_start(out=hblk[:, 0, 0:2048], out_offset=None, in_=h_all[:, 0:2048],
                                in_offset=bass.IndirectOffsetOnAxis(ap=C.idx[:, 5:6], axis=0)), reads=[rhall, C.ridx], writes=[rhblk[0]])
                    C.S.barrier_all()
                    C.S.emit()
                phase_store_x(C, xT, rx, out_d)
                return nc
            if l == 0:
                phase_ret(C, (h_all, rhall), wret_d, tab, (o_my, romy))
                if stop == 2:
                    phase_store_x(C, xT, rx, out_d)
                    return nc
                for half in range(2):
                    phase_att(C, (h_all, rhall), watt_d[half], bias5_d[half], (o_my, romy), half)
            else:
                phase_sb(C, (h_all, rhall), wsb_d, m01_d, (o_my, romy))
            if stop == 3:
                phase_store_x(C, xT, rx, out_d)
                return nc
            all_gather(C, o_my, romy, o_all, roall)
            if stop == 4:
                phase_store_x(C, xT, rx, out_d)
                return nc
            if stop == 5 + 10 * l:
                dbg_h = nc.dram_tensor("dbg_h", [2 * 512, 4096], BF16, kind="ExternalOutput").ap()
                dbg_o = nc.dram_tensor("dbg_o", [2 * 1024, 2048], BF16, kind="ExternalOutput").ap()
                C.S.dma("sp", "dma:dbg", lambda e: e.dma_start(out=dbg_h, in_=h_all), reads=[rhall])
                C.S.dma("sp", "dma:dbg", lambda e: e.dma_start(out=dbg_o, in_=o_all), reads=[roall])
                C.S.final_wait("sp", ["dma:dbg"])
                phase_store_x(C, xT, rx, out_d)
                return nc
            with ExitStack() as ph:
                hT, rh, comb, rcomb = tok_bufs(ph)
                phase_load_mix(C, (o_all, roall), hT, rh, l)
                phase_wout(C, wout_d[l], hT, rh, xT, rx, mod_col(C, l, 2))
                if stop == 6 and l == 0:
                    phase_store_x(C, xT, rx, out_d)
                    return nc
                phase_norm(C, xT, rx, C.Gt[:, l, 1, :], mod_col(C, l, 3), hT, rh)
                phase_router(C, hT, rh, comb, rcomb)
                phase_moe(C, l, wg_d[l], wu_d[l], wd_d[l], hT, rh, xT, rx, comb, rcomb, mod_col(C, l, 5))
                if stop == 7 and l == 0:
                    phase_store_x(C, xT, rx, out_d)
                    return nc
                if l == 0:
                    phase_norm(C, xT, rx, C.Gt[:, 1, 0, :], mod_col(C, 1, 0), hT, rh)
                    phase_h_out(C, hT, rh, (h_my, rhmy))
                else:
                    phase_store_x(C, xT, rx, out_d)
                del C.slots[NSLOT:]
    return nc


def rope_tables():
    inv = (10000.0 ** (-np.arange(0, 128, 2, dtype=np.float32) / 128)).astype(np.float32)
    pos = np.arange(4096, dtype=np.float32)
    ang = (pos[:, None] * inv[None, :]).astype(np.float32)
    cos = np.cos(ang).astype(np.float32)
    sin = np.sin(ang).astype(np.float32)
    cosT = np.concatenate([cos.T, cos.T], 0)
    sinT = np.concatenate([-sin.T, sin.T], 0)
    cos_tok = cos.reshape(32, 128, 64).transpose(1, 0, 2)
    sin_tok = sin.reshape(32, 128, 64).transpose(1, 0, 2)
    return dict(cosT=np.ascontiguousarray(cosT), sinT=np.ascontiguousarray(sinT),
                cos_tok=np.ascontiguousarray(cos_tok), sin_tok=np.ascontiguousarray(sin_tok))

def ret_consts(heads):
    qdec = np.zeros((128, 2, 64), np.float32)
    kdec = np.zeros((128, 2), np.float32)
    cdec = np.zeros((128, 2), np.float32)
    decp = np.zeros((128, 2, 128), np.float32)
    i = np.arange(64, dtype=np.float64)
    p = np.arange(128)
    for n, h in enumerate(heads):
        g = 1.0 - 2.0 ** (-5.0 - h)
        lg = np.log(g)
        qdec[:, n, :] = np.exp(lg * (i + 1))[None, :]
        kdec[:, n] = np.exp(lg * (63 - (p % 64))) * (128 ** -0.5)
        cdec[:, n] = np.exp(lg * 64)
        jj = p[:, None]; ii = p[None, :]
        same = (jj // 64) == (ii // 64)
        e = np.abs((ii % 64) - (jj % 64)) - (ii % 64) - 1
        decp[:, n, :] = np.where(same, np.exp(lg * e) * (128 ** -0.5), 0.0)
    return qdec.reshape(128, 128), kdec, cdec, decp.reshape(128, 256)

def wret_for(w_in, heads):
    out = np.zeros((2, 1024, 768), np.float32)
    sw = np.concatenate([np.arange(64, 128), np.arange(0, 64)])
    for n, h in enumerate(heads):
        rq = w_in[:, h * 128:(h + 1) * 128]
        rk = w_in[:, 512 + h * 128:512 + (h + 1) * 128]
        rv = w_in[:, 1024 + h * 128:1024 + (h + 1) * 128]
        rg = w_in[:, 1536 + h * 128:1536 + (h + 1) * 128]
        out[n] = np.concatenate([rq, rq[:, sw], rk, rk[:, sw], rg, rv], 1)
    return out

def ret_tables(heads):
    base4 = np.zeros((2, 128, 512), np.float32)
    diag = np.zeros((2, 4, 128, 512), np.float32)
    gpow = np.zeros((128, 2, 32), np.float32)
    j = np.arange(128, dtype=np.float64)[:, None]
    i = np.arange(512, dtype=np.float64)[None, :]
    sc = 128 ** -0.5
    for n, h in enumerate(heads):
        g = 1.0 - 2.0 ** (-5.0 - h)
        lg = np.log(g)
        base4[n] = np.exp(lg * (i - j)) * sc
        for m in range(32):
            gpow[:, n, m] = np.exp(lg * 128.0 * m)
        for r in range(4):
            sj = 128 * r + j
            cs_, ct = np.floor(sj / 64), np.floor(i / 64)
            d = np.where(cs_ > ct, 0.0, np.where(cs_ == ct, np.exp(lg * np.abs(i - sj)), np.exp(lg * np.maximum(i - sj, 0))))
            diag[n, r] = d * sc
    return base4, diag, gpow

NEG = -30000.0
def att_bias5(rel_bias, heads):
    j = np.arange(128)[:, None, None]
    kr = np.arange(5)[None, :, None]
    i = np.arange(128)[None, None, :]
    kk = 128 * kr + j
    dist = 512 + i - kk
    idx = np.clip(dist, -63, 128) + 63
    cq = 8 + i // 64
    ck = kk // 64
    valid = (ck <= cq) & (ck >= cq - 8)
    out = np.zeros((128, len(heads), 5, 128), np.float32)
    for n, h in enumerate(heads):
        g = rel_bias[h][idx]
        out[:, n] = np.where(valid, g, np.float32(NEG))
    return out.reshape(128, len(heads), 640)

def watt_for(w_in, heads):
    cols = []
    for base in (2048, 2560, 3072):
        for h in heads:
            cols.append(w_in[:, base + h * 64: base + (h + 1) * 64])
    return np.ascontiguousarray(np.concatenate(cols, 1))

def sb_consts():
    j = np.arange(128)[:, None]
    s = np.arange(128)[None, :]
    trineg = -(j >= s).astype(np.float32)
    omtneg = -(j < s).astype(np.float32)
    i = np.arange(512)[None, None, :]
    r = np.arange(4)[:, None, None]
    jj = np.arange(128)[None, :, None]
    m01 = ((128 * r + jj) < i).astype(np.float32)
    m01p = np.stack([np.concatenate([m01[3], m01[2]], 1), np.concatenate([m01[1], m01[0]], 1)])
    return trineg, omtneg, np.ascontiguousarray(m01p)

def wsb_for(w_in, heads8):
    out = np.zeros((4, 1024, 384), np.float32)
    for g in range(4):
        cols = []
        for base in (0, 1024, 2048):
            for h in heads8[g * 2:(g + 1) * 2]:
                cols.append(w_in[:, base + h * 64: base + (h + 1) * 64])
        out[g] = np.concatenate(cols, 1)
    return out


def idx_table(core):
    b, s = core // 2, core % 2
    p = np.arange(128)
    idx = np.zeros((128, 24), np.int32)
    for tb in range(8):
        rank = 2 * b + tb // 4
        idx[:, tb] = (rank * 4 + tb % 4) * 128 + p
    for kt in range(8):
        if kt < 4:
            rank, fb = 2 * b + kt // 2, kt % 2
        else:
            rank, fb = 2 * b + (kt - 4) // 2, 2 + (kt - 4) % 2
        idx[:, 8 + kt] = ((rank * 2 + s) * 4 + fb) * 128 + p
        rank, fb = 2 * b + kt // 4, kt % 4
        idx[:, 16 + kt] = ((rank * 2 + s) * 4 + fb) * 128 + p
    return idx


def _const_packs(inp, core):
    b, hh = core % 4, core // 4
    cstf = np.zeros((128, NCF), np.float32)
    cstf[:, CF_IDENT:CF_IDENT + 128] = np.eye(128, dtype=np.float32)
    cstf[:, CF_ONE] = 1.0
    cstf[:, CF_EPS] = EPS
    cstf[:, CF_QG] = np.tile(inp["att_q_norm_g"][0], 2)
    cstf[:, CF_QG + 1] = np.tile(inp["att_k_norm_g"][0], 2)
    base4, diag, gpow = ret_tables([2 * hh, 2 * hh + 1])
    cstf[:, CF_GPOW:CF_GPOW + 64] = gpow.reshape(128, 64)
    trineg, omtneg, m01 = sb_consts()
    cstb = np.zeros((128, NCB), np.float32)
    cstb[:, CB_IDENT:CB_IDENT + 128] = np.eye(128, dtype=np.float32)
    cstb[:, CB_ONESM:CB_ONESM + 128] = 1.0 / 1024
    cstb[:, CB_RW:CB_RW + 128] = inp["router_w"].reshape(8, 128, 16).transpose(1, 0, 2).reshape(128, 128)
    cstb[:, CB_ONES128:CB_ONES128 + 128] = 1.0 / 128
    cstb[:, CB_ONES64:CB_ONES64 + 64] = 1.0 / 64
    cstb[:, CB_ONE64:CB_ONE64 + 64] = 1.0
    cstb[:, CB_TRIN:CB_TRIN + 128] = trineg
    cstb[:, CB_OMTN:CB_OMTN + 128] = omtneg
    cstb[:, CB_ONESN:CB_ONESN + 128] = -1.0
    vecs = np.zeros((128, NV), np.float32)
    for l in range(2):
        vecs[:, V_NG + (l * 2 + 0) * 8:V_NG + (l * 2 + 0) * 8 + 8] = inp["norm1_g"][l].reshape(8, 128).T
        vecs[:, V_NG + (l * 2 + 1) * 8:V_NG + (l * 2 + 1) * 8 + 8] = inp["norm2_g"][l].reshape(8, 128).T
        vecs[:, V_ADAB + l * 48:V_ADAB + (l + 1) * 48] = inp["ada_b"][l].reshape(48, 128).T
    vecs[:, V_RB:V_RB + 16] = inp["router_b"][None, :]
    vecs[:, V_SEL + hh] = 1.0
    return dict(cstf=cstf, cstb=cstb, vecs=vecs, base4=base4, diag=diag, m01=m01)


_NC = []
_STOP = None
_LAST = None
_EXEC = None
_RUNKW = {}


def kernel(**inputs):
    inp = {k: np.ascontiguousarray(np.asarray(v, dtype=np.float32)) for k, v in inputs.items()}
    x = inp["x"]
    tabs = rope_tables()
    if not _NC:
        _NC.append(build_fused(_STOP))
    nc = _NC[0]
    w_in0 = inp["even_w_in"][0]
    w_in1 = inp["odd_w_in"][0]
    in_maps = []
    for c in range(8):
        b, i = c % 4, c // 4
        pk = _const_packs(inp, c)
        m = {"x_own": np.ascontiguousarray(x[b, i * T:(i + 1) * T]),
             "cvec": np.ascontiguousarray(inp["c"][b].reshape(8, 128).T),
             "ada_w": inp["ada_w"], "vecs": pk["vecs"], "cstb": pk["cstb"], "cstf": pk["cstf"],
             "wret": wret_for(w_in0, [2 * i, 2 * i + 1]),
             "watt": np.stack([watt_for(w_in0, [4 * i + 2 * hf, 4 * i + 2 * hf + 1]) for hf in range(2)]),
             "bias5": np.stack([att_bias5(inp["att_rel_bias"][0], [4 * i + 2 * hf, 4 * i + 2 * hf + 1]) for hf in range(2)]),
             "cosT": tabs["cosT"], "sinT": tabs["sinT"], "base4": pk["base4"], "diag": pk["diag"], "m01": pk["m01"],
             "wsb": wsb_for(w_in1, [8 * i + j for j in range(8)]),
             "wout0": inp["even_w_out"][0], "wout1": inp["odd_w_out"][0]}
        for l in range(2):
            m["wg%d" % l] = inp["exp_w_gate"][l]
            m["wu%d" % l] = inp["exp_w_up"][l]
            m["wd%d" % l] = inp["exp_w_down"][l]
        in_maps.append(m)
    res = run_bass_kernel_spmd(nc, in_maps, core_ids=list(range(8)), **_RUNKW)
    global _LAST, _EXEC
    _EXEC = res.exec_time_ns
    global _LAST
    _LAST = res.results
    out = np.zeros((4, SEQ, D), np.float32)
    for c in range(8):
        b, i = c % 4, c // 4
        out[b, i * T:(i + 1) * T] = res.results[c]["x_out"]
    return out
```
